# Optimizing a Trainium2 kernel written in Bass

```python
import jax, jax.numpy as jnp
from jax import lax
import numpy as np

D_MODEL = 1024
BATCH = 8
SEQ = 8192
DEPTH = 1

GRID_W = 64
CTX_LEN = 256
D_RNN = 1024
RNN_BLOCKS = 8
RNN_BW = D_RNN // RNN_BLOCKS
CONV_W = 4
CONV_LEFT = 2
LRU_C = 8.0
RET_HEADS = 8
RET_DK = 128
RET_DV = 256
RET_CHUNK = 128
D_QK = RET_HEADS * RET_DK
D_V = RET_HEADS * RET_DV
ROPE_BASE = 10000.0
IN_SPLITS = (D_RNN, D_RNN, D_QK, D_QK, D_V, D_V, D_MODEL, D_MODEL)
D_IN = sum(IN_SPLITS)
N_EXPERTS = 32
TOP_K = 4
D_EXPERT = 1024
SWIGLU_ALPHA = 1.702
SWIGLU_LIMIT = 7.0
MOE_BLOCK = 512
EPS = 1e-6

kernel_name = "hybrid_rglru_retention_moe_prefix_dit_layer"


def rmsnorm(x, g):
    xf = x.astype(jnp.float32)
    y = xf * lax.rsqrt(jnp.mean(xf * xf, axis=-1, keepdims=True) + EPS)
    return (y * g.astype(jnp.float32)).astype(x.dtype)


def modulate(h, shift, scale):
    return h * (1.0 + scale) + shift


def split_columns(z):
    points = [int(p) for p in np.cumsum(IN_SPLITS)[:-1]]
    return jnp.split(z, points, axis=-1)


def centred_depthwise_conv(x, w, b):
    T = x.shape[1]
    xp = jnp.pad(x, ((0, 0), (CONV_LEFT, CONV_W - 1 - CONV_LEFT), (0, 0)))
    y = b
    for k in range(CONV_W):
        y = y + xp[:, k:k + T] * w[k]
    return y


def _linear_combine(e1, e2):
    a1, b1 = e1
    a2, b2 = e2
    return a1 * a2, a2 * b1 + b2


def rglru_scan(xc, wa, ba, wx, bx, lam, h0, reverse):
    B, T, _ = xc.shape
    xb = xc.reshape(B, T, RNN_BLOCKS, RNN_BW)
    r = jax.nn.sigmoid(jnp.einsum('btnd,nde->btne', xb, wa).reshape(B, T, D_RNN) + ba)
    i = jax.nn.sigmoid(jnp.einsum('btnd,nde->btne', xb, wx).reshape(B, T, D_RNN) + bx)
    log_a = -LRU_C * r * jax.nn.softplus(-lam)
    a = jnp.exp(log_a)
    u = jnp.sqrt(-jnp.expm1(2.0 * log_a)) * (i * xc)
    a_cum, h = lax.associative_scan(_linear_combine, (a, u), axis=1, reverse=reverse)
    return h + a_cum * h0[:, None, :]


def axial_rope(t, rows, cols):
    n_freq = RET_DK // 4
    inv = ROPE_BASE ** (-jnp.arange(n_freq, dtype=jnp.float32) / n_freq)
    ang = jnp.concatenate([rows[:, None] * inv, cols[:, None] * inv], axis=-1)
    cos, sin = jnp.cos(ang), jnp.sin(ang)
    t1, t2 = t[..., 0::2], t[..., 1::2]
    return jnp.stack([t1 * cos - t2 * sin, t1 * sin + t2 * cos], axis=-1).reshape(t.shape)


def retention_context(q, k, v, log_g):
    L = q.shape[2]
    pos = jnp.arange(L, dtype=jnp.float32)
    decay = jnp.exp(jnp.abs(pos[:, None] - pos[None, :])[None] * log_g[:, None, None])
    scores = jnp.einsum('bhid,bhjd->bhij', q, k) * decay
    o = jnp.einsum('bhij,bhjv->bhiv', scores, v)
    k_f = k * jnp.exp((L - 1.0 - pos)[None, :] * log_g[:, None])[..., None]
    k_b = k * jnp.exp(pos[None, :] * log_g[:, None])[..., None]
    s_fwd = jnp.einsum('bhjd,bhjv->bhdv', k_f, v)
    s_bwd = jnp.einsum('bhjd,bhjv->bhdv', k_b, v)
    return o, s_fwd, s_bwd


def retention_chunked(q, k, v, s0, log_g, strict):
    B, H, S, dk = q.shape
    dv = v.shape[-1]
    C = RET_CHUNK
    n = S // C
    idx = jnp.arange(C, dtype=jnp.float32)
    diff = idx[:, None] - idx[None, :]
    mask = (diff > 0) if strict else (diff >= 0)
    intra = jnp.where(mask[None], jnp.exp(jnp.where(mask, diff, 0.0)[None] * log_g[:, None, None]), 0.0)
    q_dec = jnp.exp((idx + 1.0)[None, :] * log_g[:, None])[..., None]
    k_dec = jnp.exp((C - 1.0 - idx)[None, :] * log_g[:, None])[..., None]
    c_dec = jnp.exp(C * log_g)[:, None, None]

    def to_chunks(t):
        return jnp.moveaxis(t.reshape(B, H, n, C, t.shape[-1]), 2, 0)

    def step(state, blk):
        qb, kb, vb = blk
        s = jnp.einsum('bhid,bhjd->bhij', qb, kb) * intra
        o = jnp.einsum('bhij,bhjv->bhiv', s, vb) + jnp.einsum('bhid,bhdv->bhiv', qb * q_dec, state)
        state = state * c_dec + jnp.einsum('bhjd,bhjv->bhdv', kb * k_dec, vb)
        return state, o

    _, o = lax.scan(step, s0, (to_chunks(q), to_chunks(k), to_chunks(v)))
    return jnp.moveaxis(o, 0, 2).reshape(B, H, S, dv)


def head_groupnorm(o):
    mu = jnp.mean(o, axis=-1, keepdims=True)
    var = jnp.mean(jnp.square(o - mu), axis=-1, keepdims=True)
    return (o - mu) * lax.rsqrt(var + EPS)


def token_mixing(h_lat, h_ctx, rows, cols, w_in, conv_w, conv_b, lru_wa, lru_ba, lru_wx, lru_bx,
                 lru_lambda, w_rnn_proj, w_ret_proj, w_out, with_ctx_out):
    f32 = jnp.float32
    B, S, _ = h_lat.shape
    L = h_ctx.shape[1]
    xr_l, gr_l, q_l, k_l, v_l, gs_l, ga_l, gb_l = split_columns(h_lat @ w_in)
    xr_c, gr_c, q_c, k_c, v_c, gs_c, ga_c, gb_c = split_columns(h_ctx @ w_in)

    xr_c = centred_depthwise_conv(xr_c.astype(f32), conv_w, conv_b)
    xr_l = centred_depthwise_conv(xr_l.astype(f32), conv_w, conv_b)
    zeros = jnp.zeros((B, D_RNN), f32)
    rnn_l = 0.0
    rnn_c = 0.0
    for d in range(2):
        rev = d == 1
        hc = rglru_scan(xr_c, lru_wa[d], lru_ba[d], lru_wx[d], lru_bx[d], lru_lambda[d], zeros, rev)
        h0 = hc[:, 0] if rev else hc[:, -1]
        rnn_l = rnn_l + rglru_scan(xr_l, lru_wa[d], lru_ba[d], lru_wx[d], lru_bx[d], lru_lambda[d], h0, rev)
        rnn_c = rnn_c + hc

    log_g = jnp.log1p(-jnp.exp2(-5.0 - jnp.arange(RET_HEADS, dtype=f32)))
    k_scale = RET_DK ** -0.5

    def heads(t, dh):
        return jnp.swapaxes(t.astype(f32).reshape(B, t.shape[1], RET_HEADS, dh), 1, 2)

    qh_l = axial_rope(heads(q_l, RET_DK), rows, cols)
    kh_l = axial_rope(heads(k_l, RET_DK), rows, cols) * k_scale
    vh_l = heads(v_l, RET_DV)
    o_c, s_fwd, s_bwd = retention_context(heads(q_c, RET_DK), heads(k_c, RET_DK) * k_scale,
                                          heads(v_c, RET_DV), log_g)
    o_l = retention_chunked(qh_l, kh_l, vh_l, s_fwd, log_g, False) + jnp.flip(
        retention_chunked(jnp.flip(qh_l, 2), jnp.flip(kh_l, 2), jnp.flip(vh_l, 2), s_bwd, log_g, True), 2)

    def finish(rnn, gr, o, gs, ga, gb):
        T = o.shape[2]
        y_rnn = (rnn * jax.nn.gelu(gr.astype(f32))) @ w_rnn_proj
        ret = jnp.swapaxes(head_groupnorm(o), 1, 2).reshape(B, T, D_V)
        y_ret = (jax.nn.silu(gs.astype(f32)) * ret) @ w_ret_proj
        merged = jax.nn.sigmoid(ga.astype(f32)) * y_rnn + jax.nn.sigmoid(gb.astype(f32)) * y_ret
        return merged @ w_out

    out_l = finish(rnn_l, gr_l, o_l, gs_l, ga_l, gb_l)
    out_c = finish(rnn_c, gr_c, o_c, gs_c, ga_c, gb_c) if with_ctx_out else None
    return out_l, out_c


def moe_ffn(h, router_w, router_b, w1, b1, w2, b2):
    N, D = h.shape
    logits = (h @ router_w + router_b).astype(jnp.float32)
    top_val, top_idx = lax.top_k(logits, TOP_K)
    gates = jax.nn.softmax(top_val, axis=-1)
    flat_e = top_idx.reshape(-1).astype(jnp.int32)
    NK = flat_e.shape[0]
    order = jnp.argsort(flat_e).astype(jnp.int32)
    sorted_e = flat_e[order]
    counts = jnp.zeros((N_EXPERTS,), jnp.int32).at[flat_e].add(1)
    padded = (counts + MOE_BLOCK - 1) // MOE_BLOCK * MOE_BLOCK
    start = jnp.cumsum(counts) - counts
    pad_end = jnp.cumsum(padded)
    pad_start = pad_end - padded
    dest = pad_start[sorted_e] + jnp.arange(NK, dtype=jnp.int32) - start[sorted_e]
    n_blocks = -(-NK // MOE_BLOCK) + N_EXPERTS
    P = n_blocks * MOE_BLOCK
    row_token = jnp.full((P,), N, jnp.int32).at[dest].set(order // TOP_K)
    h_pad = jnp.concatenate([h, jnp.zeros((1, D), h.dtype)], axis=0)[row_token]
    blk_start = jnp.arange(n_blocks, dtype=jnp.int32) * MOE_BLOCK
    blk_expert = jnp.minimum(jnp.searchsorted(pad_end, blk_start, side='right'), N_EXPERTS - 1).astype(jnp.int32)

    def expert_block(args):
        hb, e = args
        gu = hb @ w1[e] + b1[e]
        glu, lin = gu[:, 0::2], gu[:, 1::2]
        glu = jnp.minimum(glu, SWIGLU_LIMIT)
        lin = jnp.clip(lin, -SWIGLU_LIMIT, SWIGLU_LIMIT)
        return (glu * jax.nn.sigmoid(SWIGLU_ALPHA * glu) * (lin + 1.0)) @ w2[e] + b2[e]

    y_pad = lax.map(expert_block, (h_pad.reshape(n_blocks, MOE_BLOCK, D), blk_expert)).reshape(P, D)
    row_of_assign = jnp.zeros((NK,), jnp.int32).at[order].set(dest)
    y = y_pad[row_of_assign].reshape(N, TOP_K, D)
    return jnp.einsum('nk,nkd->nd', gates.astype(y.dtype), y)


def setup_inputs(seed: int = 0) -> dict:
    key = jax.random.key(seed)
    ks = jax.random.split(key, 32)
    f32 = jnp.float32

    def nrm(k, shape, scale):
        return jax.random.normal(k, shape, f32) * scale

    u = jax.random.uniform(ks[14], (DEPTH, 2, D_RNN), f32, 0.9, 0.999)
    a0 = u ** (1.0 / LRU_C)
    lru_lambda = jnp.log(a0) - jnp.log1p(-a0)
    return {
        "x": nrm(ks[0], (BATCH, SEQ, D_MODEL), 1.0),
        "c": nrm(ks[1], (BATCH, D_MODEL), 1.0),
        "ctx": nrm(ks[2], (BATCH, CTX_LEN, D_MODEL), 1.0),
        "c_ctx": nrm(ks[3], (D_MODEL,), 1.0),
        "ada_w": nrm(ks[4], (DEPTH, D_MODEL, 6 * D_MODEL), 0.5 * D_MODEL ** -0.5),
        "ada_b": nrm(ks[5], (DEPTH, 6 * D_MODEL), 0.02),
        "norm1_g": 1.0 + nrm(ks[6], (DEPTH, D_MODEL), 0.05),
        "w_in": nrm(ks[7], (DEPTH, D_MODEL, D_IN), D_MODEL ** -0.5),
        "conv_w": nrm(ks[8], (DEPTH, CONV_W, D_RNN), CONV_W ** -0.5),
        "conv_b": nrm(ks[9], (DEPTH, D_RNN), 0.01),
        "lru_wa": nrm(ks[10], (DEPTH, 2, RNN_BLOCKS, RNN_BW, RNN_BW), RNN_BW ** -0.5),
        "lru_ba": nrm(ks[11], (DEPTH, 2, D_RNN), 0.01),
        "lru_wx": nrm(ks[12], (DEPTH, 2, RNN_BLOCKS, RNN_BW, RNN_BW), RNN_BW ** -0.5),
        "lru_bx": nrm(ks[13], (DEPTH, 2, D_RNN), 0.01),
        "lru_lambda": lru_lambda,
        "w_rnn_proj": nrm(ks[15], (DEPTH, D_RNN, D_MODEL), D_RNN ** -0.5),
        "w_ret_proj": nrm(ks[16], (DEPTH, D_V, D_MODEL), D_V ** -0.5),
        "w_out": nrm(ks[17], (DEPTH, D_MODEL, D_MODEL), D_MODEL ** -0.5),
        "norm2_g": 1.0 + nrm(ks[18], (DEPTH, D_MODEL), 0.05),
        "router_w": nrm(ks[19], (DEPTH, D_MODEL, N_EXPERTS), D_MODEL ** -0.5),
        "router_b": nrm(ks[20], (DEPTH, N_EXPERTS), 0.01),
        "moe_w1": nrm(ks[21], (DEPTH, N_EXPERTS, D_MODEL, 2 * D_EXPERT), D_MODEL ** -0.5),
        "moe_b1": nrm(ks[22], (DEPTH, N_EXPERTS, 2 * D_EXPERT), 0.01),
        "moe_w2": nrm(ks[23], (DEPTH, N_EXPERTS, D_EXPERT, D_MODEL), D_EXPERT ** -0.5),
        "moe_b2": nrm(ks[24], (DEPTH, N_EXPERTS, D_MODEL), 0.01),
        "final_g": 1.0 + nrm(ks[25], (D_MODEL,), 0.05),
    }


def reference(x, c, ctx, c_ctx, ada_w, ada_b, norm1_g, w_in, conv_w, conv_b, lru_wa, lru_ba, lru_wx,
              lru_bx, lru_lambda, w_rnn_proj, w_ret_proj, w_out, norm2_g, router_w, router_b,
              moe_w1, moe_b1, moe_w2, moe_b2, final_g):
    B, S, D = x.shape
    ROWS = S // GRID_W
    rows = jnp.repeat(jnp.arange(ROWS, dtype=jnp.float32), GRID_W)
    cols = jnp.tile(jnp.arange(GRID_W, dtype=jnp.float32), ROWS)
    h_c = ctx
    for layer in range(DEPTH):
        last = layer == DEPTH - 1
        mod_l = jnp.split(jax.nn.silu(c) @ ada_w[layer] + ada_b[layer], 6, axis=-1)
        sh1, sc1, g1, sh2, sc2, g2 = [m[:, None, :] for m in mod_l]
        mod_c = jnp.split(jax.nn.silu(c_ctx) @ ada_w[layer] + ada_b[layer], 6, axis=-1)
        csh1, csc1, cg1, csh2, csc2, cg2 = [m[None, None, :] for m in mod_c]

        hl = modulate(rmsnorm(x, norm1_g[layer]), sh1, sc1)
        hc = modulate(rmsnorm(h_c, norm1_g[layer]), csh1, csc1)
        y_l, y_c = token_mixing(hl, hc, rows, cols, w_in[layer], conv_w[layer], conv_b[layer],
                                lru_wa[layer], lru_ba[layer], lru_wx[layer], lru_bx[layer],
                                lru_lambda[layer], w_rnn_proj[layer], w_ret_proj[layer], w_out[layer],
                                not last)
        x = (x + g1 * y_l).astype(x.dtype)
        hl2 = modulate(rmsnorm(x, norm2_g[layer]), sh2, sc2)
        y_moe = moe_ffn(hl2.reshape(-1, D), router_w[layer], router_b[layer], moe_w1[layer],
                        moe_b1[layer], moe_w2[layer], moe_b2[layer]).reshape(B, S, D)
        x = (x + g2 * y_moe).astype(x.dtype)
        if not last:
            h_c = (h_c + cg1 * y_c).astype(h_c.dtype)
            hc2 = modulate(rmsnorm(h_c, norm2_g[layer]), csh2, csc2)
            y_cm = moe_ffn(hc2.reshape(-1, D), router_w[layer], router_b[layer], moe_w1[layer],
                           moe_b1[layer], moe_w2[layer], moe_b2[layer]).reshape(h_c.shape)
            h_c = (h_c + cg2 * y_cm).astype(h_c.dtype)
    return rmsnorm(x, final_g)
```

```python
import contextlib
import math
import numpy as np
import concourse.bass as bass
import concourse.mybir as mybir
from concourse.bass_utils import run_bass_kernel_spmd

F32 = mybir.dt.float32
F32R = mybir.dt.float32r
BF16 = mybir.dt.bfloat16
I32 = mybir.dt.int32
U32 = mybir.dt.uint32
AF = mybir.ActivationFunctionType
ALU = mybir.AluOpType
AX = mybir.AxisListType


class Res:
    __slots__ = ("w", "r", "name")

    def __init__(self, name=""):
        self.w = None
        self.r = []
        self.name = name


class Sched:
    ENG = ("pe", "act", "dve", "pool", "sp")

    def __init__(self, nc, esem, dsem):
        self.nc = nc
        self.esem = esem
        self.dsem = dsem
        self.tick = {e: 0 for e in esem}
        self.dtot = {id(s): 0 for q in dsem for s in dsem[q]}
        self.dptr = {q: 0 for q in dsem}
        self.sems = {}
        for e in esem:
            self.sems[id(esem[e])] = esem[e]
        for q in dsem:
            for s in dsem[q]:
                self.sems[id(s)] = s
        self.known = {e: {} for e in self.ENG}
        self.ops = {e: [] for e in self.ENG}
        self.nops = 0

    def _deps(self, eng, reads, writes):
        need = {}
        def add(ev):
            if ev is None:
                return
            sid, val, src = ev
            if src == "pe" and eng == "pe":
                return
            if need.get(sid, 0) < val:
                need[sid] = val
        for r in reads:
            add(r.w)
        for w in writes:
            add(w.w)
            for ev in w.r:
                add(ev)
        out = []
        kn = self.known[eng]
        for sid, val in need.items():
            if kn.get(sid, 0) < val:
                kn[sid] = val
                out.append((sid, val))
        return out

    def _mark(self, ev, reads, writes):
        for r in reads:
            r.r.append(ev)
            if len(r.r) > 64:
                r.r = r.r[-64:]
        for w in writes:
            w.w = ev
            w.r = []

    def op(self, eng, fn, reads=(), writes=()):
        waits = self._deps(eng, reads, writes)
        self.tick[eng] += 1
        sid = id(self.esem[eng])
        ev = (sid, self.tick[eng], eng)
        self.ops[eng].append((waits, fn, (sid, 1)))
        self._mark(ev, reads, writes)
        self.nops += 1
        return ev

    def dma(self, q, fn, reads=(), writes=()):
        waits = self._deps(q, reads, writes)
        pool = self.dsem[q]
        s = pool[self.dptr[q] % len(pool)]
        self.dptr[q] += 1
        sid = id(s)
        prev = self.dtot[sid]
        kn = self.known[q]
        if kn.get(sid, 0) < prev:
            kn[sid] = prev
            waits.append((sid, prev))
        self.dtot[sid] = prev + 16
        ev = (sid, prev + 16, "dma")
        self.ops[q].append((waits, fn, (sid, 16)))
        self._mark(ev, reads, writes)
        self.nops += 1
        return ev

    def wait_all_dma(self, eng):
        waits = []
        kn = self.known[eng]
        for sid, tot in self.dtot.items():
            if kn.get(sid, 0) < tot:
                kn[sid] = tot
                waits.append((sid, tot))
        if waits:
            self.ops[eng].append((waits, None, None))

    def flush(self):
        nc = self.nc
        for q in self.dsem:
            self.wait_all_dma(q)
        ops = self.ops
        sems = self.sems

        def replay(lst, eng):
            for waits, fn, inc in lst:
                for sid, val in waits:
                    eng.wait_ge(sems[sid], val)
                if fn is None:
                    continue
                inst = fn(eng)
                if inc is not None:
                    inst.then_inc(sems[inc[0]], inc[1])

        with nc.Block() as block:
            @block.tensor
            def _(e):
                replay(ops["pe"], e)

            @block.scalar
            def _(e):
                replay(ops["act"], e)

            @block.vector
            def _(e):
                replay(ops["dve"], e)

            @block.gpsimd
            def _(e):
                replay(ops["pool"], e)

            @block.sync
            def _(e):
                replay(ops["sp"], e)
        self.ops = {e: [] for e in self.ENG}
        for e in self.ENG:
            kn = self.known[e]
            for sid, tot in self.dtot.items():
                kn[sid] = tot
            for c in self.esem:
                kn[id(self.esem[c])] = self.tick[c]


D = 1024
SEQ = 8192
LC = 256
ST = SEQ + LC
NH = 8
DK = 128
DV = 256
NE = 32
BLK = 512
BLK_SHIFT = 9
NBLK = (SEQ * 4) // BLK + NE
NSLOT = NBLK * BLK
EPS = 1e-6
NCORES = 8


class Ring:
    def __init__(self, st, nc, name, n, shape, dt, psum=False):
        alloc = nc.psum_tensor if psum else nc.sbuf_tensor
        self.t = [st.enter_context(alloc(f"s_{name}{i}", shape, dt)) for i in range(n)]
        self.r = [Res(f"{name}{i}") for i in range(n)]
        self.i = 0

    def next(self):
        k = self.i % len(self.t)
        self.i += 1
        return self.t[k], self.r[k]


def build_program(debug=False, stop_after=None):
    nc = bass.Bass("TRN2", target_bir_lowering=False)
    dbg = set(debug) if debug else set()
    NEW = NE if (stop_after is None or stop_after >= "G") else 1

    def din(name, shape, dt=F32):
        return nc.dram_tensor(name, list(shape), dt, kind="ExternalInput").ap()

    def dscr(name, shape, dt=F32):
        return nc.dram_tensor(name, list(shape), dt, kind="ExternalOutput" if name in dbg else "Internal").ap()

    x_d = din("x", [SEQ, D])
    ctx_d = din("ctx", [LC, D])
    cT2_d = din("cT2", [128, 8, 2])
    ada_w_d = din("ada_w", [D, 6 * D])
    ada_b_row_d = din("ada_b_row", [1, 6 * D])
    ada_bT_d = din("ada_bT", [128, 48])
    n1gT_d = din("n1gT", [128, 8])
    n2g_row_d = din("n2g_row", [1, D])
    fg_row_d = din("fg_row", [1, D])
    w_in_d = din("w_in", [D, 10 * D])
    convT_d = din("convT", [128, 8, 5])
    lru_w_d = din("lru_w", [2, 2, 8, 128, 128])
    lru_vT_d = din("lru_vT", [128, 2, 8, 3])
    w_rnn_d = din("w_rnn", [D, D])
    w_ret_d = din("w_ret", [2 * D, D])
    w_out_d = din("w_out", [D, D])
    rw_d = din("router_w", [D, NE])
    rb_row_d = din("rb_row", [1, NE])
    w1_d = din("moe_w1", [NEW * D, 2 * D])
    b12_d = din("moe_b12", [NE, 3 * D])
    w2_d = din("moe_w2", [NEW * 512, 2 * D])
    ident_d = din("ident", [128, 128])
    ropeT_d = din("ropeT", [2, 128, SEQ])
    ropeK_d = din("ropeK", [SEQ, 2, 128])
    dec_d = din("dec", [NH, 3, 128, 128])
    ksc_d = din("ksc", [128, NH, 6])
    tri_d = din("tri", [2, 128, 128])
    bvals_d = din("bvals", [1, NBLK])
    pk_d = din("pk", [128, 8])

    out_d = nc.dram_tensor("out", [SEQ, D], F32, kind="ExternalOutput").ap()

    modbc_d = dscr("modbc", [128, 6 * D])
    modT_d = dscr("modT", [128, 48, 2])
    hT_d = dscr("hT", [D, ST], BF16)
    xrT_d = dscr("xrT", [D, ST])
    geluT_d = dscr("geluT", [D, SEQ])
    qT_d = dscr("qT", [NH, 128, SEQ], BF16)
    kT_d = dscr("kT", [NH, 128, SEQ], BF16)
    ktok_d = dscr("ktok", [ST, D], BF16)
    vtok_d = dscr("vtok", [ST, 2 * D], BF16)
    sgs_d = dscr("sgs", [SEQ, 2 * D])
    sgT_d = dscr("sgT", [2, D, SEQ])
    rgT_d = dscr("rgT", [D, SEQ], BF16)
    retg_d = dscr("retg", [SEQ, 2 * D], BF16)
    x1_d = dscr("x1", [SEQ, D])
    h2_d = dscr("h2", [SEQ, D])
    xs_d = dscr("xs", [NSLOT, D], BF16)
    ys_d = dscr("ys", [NSLOT, D])
    dk_d = dscr("dkg", [128, 64, 8])

    top = contextlib.ExitStack()
    with top:
        esem = {e: top.enter_context(nc.semaphore("es_" + e)) for e in ("pe", "act", "dve", "pool")}
        dsem = {"sp": [top.enter_context(nc.semaphore(f"ds_sp{i}")) for i in range(24)],
                "pool": [top.enter_context(nc.semaphore(f"ds_pl{i}")) for i in range(12)],
                "act": [top.enter_context(nc.semaphore(f"ds_ac{i}")) for i in range(4)]}
        S = Sched(nc, esem, dsem)

        def DMA(out, in_, reads=(), writes=(), q="sp"):
            return S.dma(q, lambda e: e.dma_start(out=out, in_=in_), reads=reads, writes=writes)

        def PE(fn, r, w):
            return S.op("pe", fn, reads=r, writes=w)

        def ACT(fn, r, w):
            return S.op("act", fn, reads=r, writes=w)

        def DVE(fn, r, w):
            return S.op("dve", fn, reads=r, writes=w)

        def POOL(fn, r, w):
            return S.op("pool", fn, reads=r, writes=w)

        def mm(ps, lhsT, rhs, start, stop, r, w):
            return PE(lambda e: e.matmul(ps, lhsT=lhsT, rhs=rhs, start=start, stop=stop), r, w)

        ident = top.enter_context(nc.sbuf_tensor("s_ident", [128, 128], F32)); R_ident = Res()
        identb = top.enter_context(nc.sbuf_tensor("s_identb", [128, 128], BF16)); R_identb = Res()
        DKi = top.enter_context(nc.sbuf_tensor("s_DKi", [128, 64, 4], I32)); R_DKi = Res()
        GK = top.enter_context(nc.sbuf_tensor("s_GK", [128, 64, 4], F32)); R_GK = Res()
        DMA(ident[:], ident_d, writes=[R_ident])
        DMA(identb[:], ident_d, writes=[R_identb], q="pool")

        def rstd_from_ss(ss, rs, Rss, Rrs, n):
            ACT(lambda e: e.activation(out=rs, in_=ss, func=AF.Sqrt, scale=1.0 / n, bias=epsc[:, 0:1]), [Rss, R_epsc], [Rrs])
            DVE(lambda e: e.reciprocal(out=rs, in_=rs), [Rrs], [Rrs])

        epsc = top.enter_context(nc.sbuf_tensor("s_epsc", [128, 1], F32)); R_epsc = Res()
        POOL(lambda e: e.memset(epsc[:], EPS), [], [R_epsc])

        with contextlib.ExitStack() as st:
            T = lambda name, shape, dt=F32: st.enter_context(nc.sbuf_tensor("s_" + name, shape, dt))
            cT2 = T("cT2", [128, 8, 2]); R_cT2 = Res()
            scb = T("scb", [128, 8, 2], BF16); R_scb = Res()
            screp = T("screp", [128, 8, 128], BF16); R_screp = Res()
            adab = T("adab", [128, 6 * D]); R_adab = Res()
            adabT = T("adabT", [128, 48]); R_adabT = Res()
            modT = T("modT", [128, 48, 2]); R_modT = Res()
            n1g = T("n1g", [128, 8]); R_n1g = Res()
            A1 = T("A1", [128, 8, 2]); R_A1 = Res()
            DMA(cT2[:], cT2_d, writes=[R_cT2])
            DMA(adab[:], ada_b_row_d.broadcast_to([128, 6 * D]), writes=[R_adab])
            DMA(adabT[:], ada_bT_d, writes=[R_adabT])
            DMA(n1g[:], n1gT_d, writes=[R_n1g])
            ACT(lambda e: e.activation(out=scb[:], in_=cT2[:], func=AF.Silu), [R_cT2], [R_scb])
            for k in range(8):
                POOL(lambda e, k=k: e.tensor_copy(out=screp[:, k, :], in_=scb[:, k, 0:1].broadcast_to([128, 128])), [R_scb], [R_screp])
            wg = Ring(st, nc, "adaw", 2, [128, 8, 512], BF16)
            psA = Ring(st, nc, "psA", 2, [128, 512], F32, psum=True)
            psB = Ring(st, nc, "psB", 2, [128, 4, 2], F32, psum=True)
            mbc = Ring(st, nc, "mbc", 2, [128, 512], F32)
            ada_v = ada_w_d.rearrange("(k p) f -> p k f", p=128)
            for g in range(12):
                w, Rw = wg.next()
                DMA(w[:], ada_v[:, :, g * 512:(g + 1) * 512], writes=[Rw], q="pool")
                p1, Rp1 = psA.next()
                for k in range(8):
                    mm(p1[:], screp[:, k, :], w[:, k, :], k == 0, k == 7, [R_screp, Rw], [Rp1])
                m, Rm = mbc.next()
                DVE(lambda e, m=m, p1=p1, g=g: e.tensor_tensor(out=m[:], in0=p1[:], in1=adab[:, g * 512:(g + 1) * 512], op=ALU.add), [Rp1, R_adab], [Rm])
                DMA(modbc_d[:, g * 512:(g + 1) * 512], m[:], reads=[Rm])
                p2, Rp2 = psB.next()
                for j in range(4):
                    for k in range(8):
                        mm(p2[:, j, :], w[:, k, j * 128:(j + 1) * 128], scb[:, k, :], k == 0, k == 7, [R_scb, Rw], [Rp2])
                DVE(lambda e, p2=p2, g=g: e.tensor_tensor(out=modT[:, g * 4:(g + 1) * 4, :], in0=p2[:], in1=adabT[:, g * 4:(g + 1) * 4].unsqueeze(2).broadcast_to([128, 4, 2]), op=ALU.add), [Rp2, R_adabT], [R_modT])
            DMA(modT_d, modT[:], reads=[R_modT])
            DVE(lambda e: e.tensor_scalar(out=A1[:], in0=modT[:, 8:16, :], scalar1=1.0, scalar2=None, op0=ALU.add), [R_modT], [R_A1])
            DVE(lambda e: e.tensor_tensor(out=A1[:], in0=A1[:], in1=n1g[:].unsqueeze(2).broadcast_to([128, 8, 2]), op=ALU.mult), [R_A1, R_n1g], [R_A1])

            xin = Ring(st, nc, "xin", 4, [128, D], F32)
            sq = Ring(st, nc, "sq", 2, [128, D], BF16)
            xnb = Ring(st, nc, "xnb", 3, [128, D], BF16)
            ssr = Ring(st, nc, "ssr", 4, [128, 2], F32)
            pT = Ring(st, nc, "pT", 2, [128, 8, 128], BF16, psum=True)
            hst = Ring(st, nc, "hst", 2, [128, 8, 512], BF16)
            hT_v = hT_d.rearrange("(k p) t -> p k t", p=128)
            def stageA1(tt):
                is_ctx = tt >= 64
                src = ctx_d[(tt - 64) * 128:(tt - 63) * 128, :] if is_ctx else x_d[tt * 128:(tt + 1) * 128, :]
                xt, Rx = xin.next()
                DMA(xt[:], src, writes=[Rx])
                sqt, Rsq = sq.next()
                ss, Rss = ssr.next()
                ACT(lambda e, sqt=sqt, xt=xt, ss=ss: e.activation(out=sqt[:], in_=xt[:], func=AF.Square, accum_out=ss[:, 0:1]), [Rx], [Rsq, Rss])
                rstd_from_ss(ss[:, 0:1], ss[:, 1:2], Rss, Rss, D)
                xn, Rxn = xnb.next()
                ACT(lambda e, xn=xn, xt=xt, ss=ss: e.activation(out=xn[:], in_=xt[:], func=AF.Copy, scale=ss[:, 1:2]), [Rx, Rss], [Rxn])
                return xn, Rxn

            cur = None
            nxtA1 = stageA1(0)
            for tt in range(66):
                is_ctx = tt >= 64
                s = 1 if is_ctx else 0
                xn, Rxn = nxtA1
                if tt + 1 < 66:
                    nxtA1 = stageA1(tt + 1)
                p, Rp = pT.next()
                for k in range(8):
                    PE(lambda e, p=p, xn=xn, k=k: e.transpose(out=p[:, k, :], in_=xn[:, k * 128:(k + 1) * 128], identity=identb[:]), [Rxn, R_identb], [Rp])
                j = tt % 4
                if j == 0 or tt == 64:
                    cur = hst.next()
                    j = 0 if tt == 64 else j
                if tt == 65:
                    j = 1
                h, Rh = cur
                for k in range(8):
                    if k % 2 == 0:
                        DVE(lambda e, h=h, p=p, k=k, j=j, s=s: e.tensor_scalar(out=h[:, k, j * 128:(j + 1) * 128], in0=p[:, k, :], scalar1=A1[:, k, s:s + 1], scalar2=modT[:, k, s:s + 1], op0=ALU.mult, op1=ALU.add), [Rp, R_A1, R_modT], [Rh])
                    else:
                        ACT(lambda e, h=h, p=p, k=k, j=j, s=s: e.activation(out=h[:, k, j * 128:(j + 1) * 128], in_=p[:, k, :], func=AF.Identity, scale=A1[:, k, s:s + 1], bias=modT[:, k, s:s + 1]), [Rp, R_A1, R_modT], [Rh])
                if tt < 64 and tt % 4 == 3:
                    t0 = (tt // 4) * 512
                    DMA(hT_v[:, :, t0:t0 + 512], h[:], reads=[Rh])
                if tt == 65:
                    DMA(hT_v[:, :, SEQ:ST], h[:, :, 0:256], reads=[Rh])
            S.flush()
        if stop_after == "A":
            return nc

        with contextlib.ExitStack() as st:
            T = lambda name, shape, dt=F32: st.enter_context(nc.sbuf_tensor("s_" + name, shape, dt))
            wv = w_in_d.rearrange("(k p) f -> p k f", p=128)
            hT_v = hT_d.rearrange("(k p) t -> p k t", p=128)
            wr = Ring(st, nc, "wgrp", 2, [128, 8, 1024], BF16)
            wp, R_wp = T("wperm", [128, 8, 1024], BF16), Res()
            hin = Ring(st, nc, "hin", 3, [128, 8, 512], BF16)
            psM = Ring(st, nc, "psM", 4, [128, 512], F32, psum=True)
            psTq = Ring(st, nc, "psTq", 4, [128, 512], BF16, psum=True)
            sqk = Ring(st, nc, "sqk", 10, [128, 1024], BF16)
            stf = Ring(st, nc, "stf", 3, [128, 1024], F32)
            stb = Ring(st, nc, "stb", 3, [128, 1024], BF16)
            tmpA = Ring(st, nc, "tmpA", 3, [128, 512], F32)
            tmpB = Ring(st, nc, "tmpB", 3, [128, 512], F32)
            ropeKr = Ring(st, nc, "ropeKr", 3, [128, 2, 128], F32)
            GELU_C = 1.5957691216057308
            def ntile_of(g):
                return 17 if g in (0, 3, 4, 5) else 16

            def issue_w(g):
                W, RW = wr.next()
                DMA(W[:, 0:4, :], wv[:, 0:4, g * 1024:(g + 1) * 1024], writes=[RW], q="pool")
                DMA(W[:, 4:8, :], wv[:, 4:8, g * 1024:(g + 1) * 1024], writes=[RW], q="pool")
                return W, RW

            def issue_tile(g, tt):
                n = 256 if tt == 16 else 512
                t0 = tt * 512
                h, Rh = hin.next()
                DMA(h[:, :, 0:n], hT_v[:, :, t0:t0 + n], writes=[Rh])
                rp = Rrp = None
                return h, Rh, rp, Rrp

            def do_T(qk_tiles, dst, t0):
                for hd in range(8):
                    pq, Rpq = psTq.next()
                    for ts in range(4):
                        so, Rso = qk_tiles[ts]
                        PE(lambda e, pq=pq, so=so, ts=ts, hd=hd: e.transpose(out=pq[:, ts * 128:(ts + 1) * 128], in_=so[:, hd * 128:(hd + 1) * 128], identity=identb[:]), [Rso, R_identb], [Rpq])
                    sq_, Rsq_ = stb.next()
                    if hd % 2 == 0:
                        ACT(lambda e, sq_=sq_, pq=pq: e.activation(out=sq_[:, 0:512], in_=pq[:], func=AF.Copy), [Rpq], [Rsq_])
                    else:
                        DVE(lambda e, sq_=sq_, pq=pq: e.tensor_copy(out=sq_[:, 0:512], in_=pq[:]), [Rpq], [Rsq_])
                    DMA(dst[hd, :, t0:t0 + 512], sq_[:, 0:512], reads=[Rsq_])

            pending_qk = None
            items = [(g, tt) for g in range(10) for tt in range(ntile_of(g))]
            nxt_tile = issue_tile(*items[0])
            nxt_w = issue_w(0)
            item_i = 0
            for g in range(10):
                W, RW = nxt_w
                if g + 1 < 10:
                    nxt_w = issue_w(g + 1)
                if g in (2, 3):
                    src = W[:].rearrange("p k (h a two) -> p (k h) two a", h=8, two=2)
                    POOL(lambda e, src=src: e.tensor_copy(out=wp[:].rearrange("p k (h two a) -> p (k h) two a", h=8, two=2), in_=src), [RW], [R_wp])
                feat = g in (0, 1, 8, 9)
                tokm = g in (2, 3, 4, 5, 6, 7)
                ntile = ntile_of(g)
                for tt in range(ntile):
                    is_ctx = tt == 16
                    n = 256 if is_ctx else 512
                    t0 = tt * 512
                    h, Rh, rp, Rrp = nxt_tile
                    item_i += 1
                    if item_i < len(items):
                        nxt_tile = issue_tile(*items[item_i])
                    if feat and not (is_ctx and g != 0):
                        for fc in range(8):
                            ps, Rps = psM.next()
                            Wl, RWl = (wp, R_wp) if g in (2, 3) else (W, RW)
                            for k in range(8):
                                mm(ps[:, 0:n], Wl[:, k, fc * 128:(fc + 1) * 128], h[:, k, 0:n], k == 0, k == 7, [RWl, Rh], [Rps])
                            if g == 0:
                                so, Rso = stf.next()
                                ACT(lambda e, so=so, ps=ps, n=n: e.activation(out=so[:, 0:n], in_=ps[:, 0:n], func=AF.Copy), [Rps], [Rso])
                                DMA(xrT_d[fc * 128:(fc + 1) * 128, t0:t0 + n], so[:, 0:n], reads=[Rso])
                            elif g == 1:
                                ta, Rta = tmpA.next()
                                so, Rso = stf.next()
                                ACT(lambda e, ta=ta, ps=ps: e.activation(out=ta[:], in_=ps[:], func=AF.Square), [Rps], [Rta])
                                DVE(lambda e, ta=ta: e.tensor_scalar(out=ta[:], in0=ta[:], scalar1=0.044715, scalar2=1.0, op0=ALU.mult, op1=ALU.add), [Rta], [Rta])
                                DVE(lambda e, ta=ta, ps=ps: e.tensor_tensor(out=ta[:], in0=ta[:], in1=ps[:], op=ALU.mult), [Rta, Rps], [Rta])
                                ACT(lambda e, ta=ta: e.activation(out=ta[:], in_=ta[:], func=AF.Sigmoid, scale=GELU_C), [Rta], [Rta])
                                DVE(lambda e, ta=ta, ps=ps, so=so: e.tensor_tensor(out=so[:, 0:512], in0=ta[:], in1=ps[:], op=ALU.mult), [Rta, Rps], [Rso])
                                DMA(geluT_d[fc * 128:(fc + 1) * 128, t0:t0 + 512], so[:, 0:512], reads=[Rso])
                            elif g in (8, 9):
                                so, Rso = stf.next()
                                ACT(lambda e, so=so, ps=ps: e.activation(out=so[:, 0:512], in_=ps[:], func=AF.Sigmoid), [Rps], [Rso])
                                DMA(sgT_d[g - 8, fc * 128:(fc + 1) * 128, t0:t0 + 512], so[:, 0:512], reads=[Rso])
                            else:
                                ps2, Rps2 = psW.next()
                                for k in range(8):
                                    mm(ps2[:], ws[:, k, fc * 128:(fc + 1) * 128], h[:, k, :], k == 0, k == 7, [R_ws, Rh], [Rps2])
                                ta, Rta = tmpA.next()
                                tb, Rtb = tmpB.next()
                                so, Rso = stb.next()
                                DVE(lambda e, ta=ta, ps=ps, rp=rp: e.tensor_tensor(out=ta[:], in0=ps[:], in1=rp[:, 0, :], op=ALU.mult), [Rps, Rrp], [Rta])
                                DVE(lambda e, tb=tb, ps2=ps2, rp=rp: e.tensor_tensor(out=tb[:], in0=ps2[:], in1=rp[:, 1, :], op=ALU.mult), [Rps2, Rrp], [Rtb])
                                POOL(lambda e, ta=ta, tb=tb, so=so: e.tensor_tensor(out=so[:, 0:512], in0=ta[:], in1=tb[:], op=ALU.add), [Rta, Rtb], [Rso])
                                dst = qT_d if g == 2 else kT_d
                                DMA(dst[fc, :, t0:t0 + 512], so[:, 0:512], reads=[Rso])
                    if tokm:
                        qk_tiles = []
                        for ts in range(n // 128):
                            r0 = (SEQ + ts * 128) if is_ctx else (t0 + ts * 128)
                            if g in (2, 3) and not is_ctx:
                                rk, Rrk = ropeKr.next()
                                DMA(rk[:], ropeK_d[r0:r0 + 128, :, :], writes=[Rrk])
                            if g in (6, 7):
                                so, Rso = stf.next()
                            elif g in (2, 3):
                                so, Rso = sqk.next()
                            else:
                                so, Rso = stb.next()
                            for ch in range(2):
                                ps, Rps = psM.next()
                                Wl, RWl = (wp, R_wp) if g in (2, 3) else (W, RW)
                                for k in range(8):
                                    mm(ps[:], h[:, k, ts * 128:(ts + 1) * 128], Wl[:, k, ch * 512:(ch + 1) * 512], k == 0, k == 7, [RWl, Rh], [Rps])
                                osl = so[:, ch * 512:(ch + 1) * 512]
                                if g in (4, 5) or (g == 3 and is_ctx):
                                    ACT(lambda e, osl=osl, ps=ps: e.activation(out=osl, in_=ps[:], func=AF.Copy), [Rps], [Rso])
                                elif g in (6, 7):
                                    ACT(lambda e, osl=osl, ps=ps: e.activation(out=osl, in_=ps[:], func=AF.Silu), [Rps], [Rso])
                                else:
                                    ta, Rta = tmpA.next()
                                    tb, Rtb = tmpB.next()
                                    v4 = lambda a: a.rearrange("p (h c) -> p h c", h=4)
                                    v5 = lambda a: a.rearrange("p (h two c) -> p h two c", h=4, two=2)
                                    DVE(lambda e, ta=ta, ps=ps, rk=rk: e.tensor_tensor(out=v4(ta[:]), in0=v4(ps[:]), in1=rk[:, 0:1, :].broadcast_to([128, 4, 128]), op=ALU.mult), [Rps, Rrk], [Rta])
                                    for hh in range(2):
                                        DVE(lambda e, tb=tb, ps=ps, rk=rk, hh=hh: e.tensor_tensor(out=v5(tb[:])[:, :, hh, :], in0=v5(ps[:])[:, :, 1 - hh, :], in1=rk[:, 1:2, hh * 64:(hh + 1) * 64].broadcast_to([128, 4, 64]), op=ALU.mult), [Rps, Rrk], [Rtb])
                                    POOL(lambda e, ta=ta, tb=tb, osl=osl: e.tensor_tensor(out=osl, in0=ta[:], in1=tb[:], op=ALU.add), [Rta, Rtb], [Rso])
                            if g == 3:
                                DMA(ktok_d[r0:r0 + 128, :], so[:], reads=[Rso])
                            elif g in (4, 5):
                                DMA(vtok_d[r0:r0 + 128, (g - 4) * 1024:(g - 3) * 1024], so[:], reads=[Rso])
                            elif g in (6, 7):
                                DMA(sgs_d[r0:r0 + 128, (g - 6) * 1024:(g - 5) * 1024], so[:], reads=[Rso])
                            qk_tiles.append((so, Rso))
                        if g in (2, 3) and not is_ctx:
                            if pending_qk is not None:
                                do_T(*pending_qk)
                            pending_qk = (qk_tiles, qT_d if g == 2 else kT_d, t0)
                if pending_qk is not None:
                    do_T(*pending_qk)
                    pending_qk = None
            S.flush()
        if stop_after == "B":
            return nc

        with contextlib.ExitStack() as st:
            T = lambda name, shape, dt=F32: st.enter_context(nc.sbuf_tensor("s_" + name, shape, dt))
            X, R_X = T("X", [128, ST]), Res()
            XC, R_XC = T("XC", [128, ST]), Res()
            cv, R_cv = T("cv", [128, 8, 5]), Res()
            lv, R_lv = T("lv", [128, 2, 8, 3]), Res()
            cn, R_cn = T("cn", [128, 2, 8, 2]), Res()
            onec, R_onec = T("onec", [128, 1]), Res()
            hb, R_hb = T("hb", [128, 4]), Res()
            DMA(cv[:], convT_d, writes=[R_cv])
            DMA(lv[:], lru_vT_d, writes=[R_lv])
            POOL(lambda e: e.memset(onec[:], 1.0), [], [R_onec])
            ACT(lambda e: e.activation(out=cn[:, :, :, 0], in_=lv[:, :, :, 2], func=AF.Exp, scale=-1.0), [R_lv], [R_cn])
            ACT(lambda e: e.activation(out=cn[:, :, :, 0], in_=cn[:, :, :, 0], func=AF.Ln, bias=onec[:, 0:1]), [R_cn, R_onec], [R_cn])
            DVE(lambda e: e.tensor_scalar(out=cn[:, :, :, 1], in0=cn[:, :, :, 0], scalar1=-16.0, scalar2=None, op0=ALU.mult), [R_cn], [R_cn])
            DVE(lambda e: e.tensor_scalar(out=cn[:, :, :, 0], in0=cn[:, :, :, 0], scalar1=-8.0, scalar2=None, op0=ALU.mult), [R_cn], [R_cn])
            wl = Ring(st, nc, "wl", 2, [128, 4, 128], BF16)
            xcb = Ring(st, nc, "xcb", 2, [128, 2048], BF16)
            Rr = Ring(st, nc, "Rr", 2, [128, 2048], F32)
            Ir = Ring(st, nc, "Ir", 2, [128, 2048], F32)
            Ar = Ring(st, nc, "Ar", 2, [128, 2048], F32)
            Ur = Ring(st, nc, "Ur", 2, [128, 2048], F32)
            Gr = Ring(st, nc, "Gr", 2, [128, 2048], F32)
            Or = Ring(st, nc, "Or", 2, [128, 2048], BF16)
            psR = Ring(st, nc, "psR", 3, [128, 512], F32, psum=True)
            psI = Ring(st, nc, "psI", 3, [128, 512], F32, psum=True)
            lat = [(i * 2048, (i + 1) * 2048) for i in range(4)]
            for n in range(8):
                DMA(X[:], xrT_d[n * 128:(n + 1) * 128, :], writes=[R_X])
                w, Rw = wl.next()
                DMA(w[:], lru_w_d[:, :, n, :, :].rearrange("d a i o -> i (d a) o"), writes=[Rw], q="pool")
                ACT(lambda e, n=n: e.activation(out=XC[:], in_=X[:], func=AF.Identity, scale=cv[:, n, 2:3], bias=cv[:, n, 4:5]), [R_X, R_cv], [R_XC])
                for (s, e_) in ((0, SEQ), (SEQ, ST)):
                    for k, off in ((0, -2), (1, -1), (3, 1)):
                        if off < 0:
                            o_sl, i_sl = slice(s - off, e_), slice(s, e_ + off)
                        else:
                            o_sl, i_sl = slice(s, e_ - off), slice(s + off, e_)
                        DVE(lambda e, n=n, k=k, o_sl=o_sl, i_sl=i_sl: e.scalar_tensor_tensor(out=XC[:, o_sl], in0=X[:, i_sl], scalar=cv[:, n, k:k + 1], in1=XC[:, o_sl], op0=ALU.mult, op1=ALU.add), [R_X, R_XC, R_cv], [R_XC])
                work = []
                for d in range(2):
                    order = [(SEQ, ST)] + (lat if d == 0 else lat[::-1])
                    for i_, (s, e_) in enumerate(order):
                        work.append((d, s, e_, i_ == 0))

                def stage1(d, s, e_, first, n=n, w=w, Rw=Rw):
                    L = e_ - s
                    xb, Rxb = xcb.next()
                    DVE(lambda e, xb=xb, s=s, e_=e_, L=L: e.tensor_copy(out=xb[:, 0:L], in_=XC[:, s:e_]), [R_XC], [Rxb])
                    Rb, RRb = Rr.next()
                    Ib, RIb = Ir.next()
                    Ab, RAb = Ar.next()
                    for pc in range(0, L, 512):
                        m = min(512, L - pc)
                        pr, Rpr = psR.next()
                        pi, Rpi = psI.next()
                        mm(pr[:, 0:m], w[:, d * 2 + 0, :], xb[:, pc:pc + m], True, True, [Rw, Rxb], [Rpr])
                        mm(pi[:, 0:m], w[:, d * 2 + 1, :], xb[:, pc:pc + m], True, True, [Rw, Rxb], [Rpi])
                        ACT(lambda e, Rb=Rb, pr=pr, pc=pc, m=m, d=d, n=n: e.activation(out=Rb[:, pc:pc + m], in_=pr[:, 0:m], func=AF.Sigmoid, bias=lv[:, d, n, 0:1]), [Rpr, R_lv], [RRb])
                        ACT(lambda e, Ib=Ib, pi=pi, pc=pc, m=m, d=d, n=n: e.activation(out=Ib[:, pc:pc + m], in_=pi[:, 0:m], func=AF.Sigmoid, bias=lv[:, d, n, 1:2]), [Rpi, R_lv], [RIb])
                    ACT(lambda e, Ab=Ab, Rb=Rb, L=L, d=d, n=n: e.activation(out=Ab[:, 0:L], in_=Rb[:, 0:L], func=AF.Exp, scale=cn[:, d, n, 0:1]), [RRb, R_cn], [RAb])
                    ACT(lambda e, Rb=Rb, L=L, d=d, n=n: e.activation(out=Rb[:, 0:L], in_=Rb[:, 0:L], func=AF.Exp, scale=cn[:, d, n, 1:2]), [RRb, R_cn], [RRb])
                    ACT(lambda e, Rb=Rb, L=L: e.activation(out=Rb[:, 0:L], in_=Rb[:, 0:L], func=AF.Sqrt, scale=-1.0, bias=onec[:, 0:1]), [RRb, R_onec], [RRb])
                    return (Rb, RRb, Ib, RIb, Ab, RAb)

                def stage2(d, s, e_, first, bufs, n=n):
                    Rb, RRb, Ib, RIb, Ab, RAb = bufs
                    L = e_ - s
                    is_ctx = s == SEQ
                    Ub, RUb = Ur.next()
                    DVE(lambda e, Ub=Ub, Rb=Rb, Ib=Ib, L=L: e.tensor_tensor(out=Ub[:, 0:L], in0=Rb[:, 0:L], in1=Ib[:, 0:L], op=ALU.mult), [RRb, RIb], [RUb])
                    DVE(lambda e, Ub=Ub, s=s, e_=e_, L=L: e.tensor_tensor(out=Ub[:, 0:L], in0=Ub[:, 0:L], in1=XC[:, s:e_], op=ALU.mult), [RUb, R_XC], [RUb])
                    init = 0.0 if first else hb[:, d:d + 1]
                    if d == 0:
                        dst, Rdst = (Ib, RIb) if is_ctx else (X, R_X)
                        dsl = dst[:, 0:L] if is_ctx else dst[:, s:e_]
                        DVE(lambda e, dsl=dsl, Ab=Ab, Ub=Ub, L=L, init=init: e.tensor_tensor_scan(out=dsl, data0=Ab[:, 0:L], data1=Ub[:, 0:L], initial=init, op0=ALU.mult, op1=ALU.add), [RAb, RUb, R_hb], [Rdst])
                        last = dst[:, L - 1:L] if is_ctx else dst[:, e_ - 1:e_]
                        DVE(lambda e, last=last, d=d: e.tensor_copy(out=hb[:, d:d + 1], in_=last), [Rdst], [R_hb])
                    else:
                        dst, Rdst = Ib, RIb
                        DVE(lambda e, dst=dst, Ab=Ab, Ub=Ub, L=L, init=init: e.tensor_tensor_scan(out=dst[:, 0:L][:, ::-1], data0=Ab[:, 0:L][:, ::-1], data1=Ub[:, 0:L][:, ::-1], initial=init, op0=ALU.mult, op1=ALU.add), [RAb, RUb, R_hb], [Rdst])
                        DVE(lambda e, dst=dst, d=d: e.tensor_copy(out=hb[:, d:d + 1], in_=dst[:, 0:1]), [Rdst], [R_hb])
                        if not is_ctx:
                            G, RG = Gr.next()
                            DMA(G[:], geluT_d[n * 128:(n + 1) * 128, s:e_], writes=[RG])
                            DVE(lambda e, dst=dst, s=s, e_=e_: e.tensor_tensor(out=dst[:, 0:2048], in0=dst[:, 0:2048], in1=X[:, s:e_], op=ALU.add), [Rdst, R_X], [Rdst])
                            O, RO = Or.next()
                            POOL(lambda e, O=O, dst=dst, G=G: e.tensor_tensor(out=O[:], in0=dst[:, 0:2048], in1=G[:], op=ALU.mult), [Rdst, RG], [RO])
                            DMA(rgT_d[n * 128:(n + 1) * 128, s:e_], O[:], reads=[RO])

                nxt_b = stage1(*work[0])
                for wi, item in enumerate(work):
                    cur_b = nxt_b
                    if wi + 1 < len(work):
                        nxt_b = stage1(*work[wi + 1])
                    stage2(*item, cur_b)
            S.flush()
        if stop_after == "C":
            return nc

        with contextlib.ExitStack() as st:
            T = lambda name, shape, dt=F32: st.enter_context(nc.sbuf_tensor("s_" + name, shape, dt))
            cdec = _const_tables()["cdec"]
            QT, R_QT = T("QT", [128, SEQ], BF16), Res()
            KT, R_KT = T("KT", [128, SEQ], BF16), Res()
            Vt, R_Vt = T("Vt", [128, 66, 256], BF16), Res()
            Kf, R_Kf = T("Kf", [128, 66, 128], BF16), Res()
            Kb, R_Kb = T("Kb", [128, 66, 128], BF16), Res()
            SBs, R_SBs = T("SBs", [128, 64, 256], BF16), Res()
            SFs, R_SFs = T("SFs", [128, 64, 256], BF16), Res()
            dech, R_dech = T("dech", [128, 3, 128]), Res()
            ksc, R_ksc = T("ksc", [128, NH, 6]), Res()
            DMA(ksc[:], ksc_d, writes=[R_ksc])
            SFm = Ring(st, nc, "SFm", 2, [128, 256], F32)
            SBm = Ring(st, nc, "SBm", 2, [128, 256], F32)
            Ar = Ring(st, nc, "Ach", 8, [128, 128], BF16)
            qfr = Ring(st, nc, "qfr", 8, [128, 128], BF16)
            qbr = Ring(st, nc, "qbr", 8, [128, 128], BF16)
            st6 = Ring(st, nc, "st6", 6, [128, 6], F32)
            mvr = Ring(st, nc, "mvr", 3, [128, 4, 4], F32)
            RET = Ring(st, nc, "RET", 2, [128, 4, 256], F32)
            SGS = Ring(st, nc, "SGS", 2, [128, 4, 256], F32)
            RGo = Ring(st, nc, "RGo", 2, [128, 4, 256], BF16)
            psA = Ring(st, nc, "psDa", 2, [128, 512], F32, psum=True)
            psO = Ring(st, nc, "psDo", 6, [128, 512], F32, psum=True)
            v_v = vtok_d.rearrange("(n j) f -> j n f", j=128)
            k_v = ktok_d.rearrange("(n j) f -> j n f", j=128)
            for h in range(NH):
                DMA(Vt[:], v_v[:, :, h * 256:(h + 1) * 256], writes=[R_Vt])
                DMA(Kf[:], k_v[:, :, h * 128:(h + 1) * 128], writes=[R_Kf])
                DMA(Kb[:], k_v[:, :, h * 128:(h + 1) * 128], writes=[R_Kb])
                DMA(QT[:], qT_d[h], writes=[R_QT])
                DMA(KT[:], kT_d[h], writes=[R_KT])
                DMA(dech[:], dec_d[h].rearrange("c p i -> p c i"), writes=[R_dech])
                ACT(lambda e, h=h: e.activation(out=Kf[:, 0:64, :], in_=Kf[:, 0:64, :], func=AF.Copy, scale=ksc[:, h, 0:1]), [R_Kf, R_ksc], [R_Kf])
                DVE(lambda e, h=h: e.tensor_scalar(out=Kb[:, 0:64, :], in0=Kb[:, 0:64, :], scalar1=ksc[:, h, 1:2], scalar2=None, op0=ALU.mult), [R_Kb, R_ksc], [R_Kb])
                for (Kx, RKx, c0) in ((Kf, R_Kf, 0), (Kb, R_Kb, 1)):
                    for c in range(2):
                        col = 2 + c0 * 2 + c
                        ACT(lambda e, Kx=Kx, c=c, col=col, h=h: e.activation(out=Kx[:, 64 + c, :], in_=Kx[:, 64 + c, :], func=AF.Copy, scale=ksc[:, h, col:col + 1]), [RKx, R_ksc], [RKx])
                sf, Rsf = SFm.next()
                sb, Rsb = SBm.next()
                for (Kx, RKx, Sm, RSm) in ((Kf, R_Kf, sf, Rsf), (Kb, R_Kb, sb, Rsb)):
                    pu, Rpu = psA.next()
                    for c in range(2):
                        mm(pu[:, 0:256], Kx[:, 64 + c, :], Vt[:, 64 + c, :], c == 0, c == 1, [RKx, R_Vt], [Rpu])
                    DVE(lambda e, Sm=Sm, pu=pu: e.tensor_copy(out=Sm[:], in_=pu[:, 0:256]), [Rpu], [RSm])
                for i in range(64):
                    ACT(lambda e, i=i, sf=sf: e.activation(out=SFs[:, i, :], in_=sf[:], func=AF.Copy), [Rsf], [R_SFs])
                    if i < 63:
                        pu, Rpu = psA.next()
                        mm(pu[:, 0:256], Kf[:, i, :], Vt[:, i, :], True, True, [R_Kf, R_Vt], [Rpu])
                        sf2, Rsf2 = SFm.next()
                        DVE(lambda e, sf2=sf2, sf=sf, pu=pu, h=h: e.scalar_tensor_tensor(out=sf2[:], in0=sf[:], scalar=cdec[h], in1=pu[:, 0:256], op0=ALU.mult, op1=ALU.add), [Rsf, Rpu], [Rsf2])
                        sf, Rsf = sf2, Rsf2
                    n = 63 - i
                    POOL(lambda e, n=n, sb=sb: e.tensor_copy(out=SBs[:, n, :], in_=sb[:]), [Rsb], [R_SBs])
                    if n > 0:
                        pu, Rpu = psA.next()
                        mm(pu[:, 0:256], Kb[:, n, :], Vt[:, n, :], True, True, [R_Kb, R_Vt], [Rpu])
                        sb2, Rsb2 = SBm.next()
                        DVE(lambda e, sb2=sb2, sb=sb, pu=pu, h=h: e.scalar_tensor_tensor(out=sb2[:], in0=sb[:], scalar=cdec[h], in1=pu[:, 0:256], op0=ALU.mult, op1=ALU.add), [Rsb, Rpu], [Rsb2])
                        sb, Rsb = sb2, Rsb2
                def stageA(g4):
                    lst = []
                    for jj in range(4):
                        n = g4 * 4 + jj
                        sl = slice(n * 128, (n + 1) * 128)
                        ps, Rps = psA.next()
                        mm(ps[:, 0:128], KT[:, sl], QT[:, sl], True, True, [R_KT, R_QT], [Rps])
                        A, RA = Ar.next()
                        DVE(lambda e, A=A, ps=ps: e.tensor_tensor(out=A[:], in0=ps[:, 0:128], in1=dech[:, 0, :], op=ALU.mult), [Rps, R_dech], [RA])
                        qf, Rqf = qfr.next()
                        qb, Rqb = qbr.next()
                        POOL(lambda e, qf=qf, sl=sl: e.tensor_tensor(out=qf[:], in0=QT[:, sl], in1=dech[:, 1, :], op=ALU.mult), [R_QT, R_dech], [Rqf])
                        POOL(lambda e, qb=qb, sl=sl: e.tensor_tensor(out=qb[:], in0=QT[:, sl], in1=dech[:, 2, :], op=ALU.mult), [R_QT, R_dech], [Rqb])
                        lst.append((A, RA, qf, Rqf, qb, Rqb))
                    return lst

                nxtA = stageA(0)
                for g4 in range(16):
                    n0 = g4 * 4
                    curA = nxtA
                    if g4 + 1 < 16:
                        nxtA = stageA(g4 + 1)
                    ret, Rret = RET.next()
                    sg, Rsg = SGS.next()
                    DMA(sg[:], sgs_d[n0 * 128:(n0 + 4) * 128, h * 256:(h + 1) * 256].rearrange("(c i) f -> i c f", i=128), writes=[Rsg])
                    mv, Rmv = mvr.next()
                    pos = []
                    for jj in range(4):
                        n = n0 + jj
                        A, RA, qf, Rqf, qb, Rqb = curA[jj]
                        po, Rpo = psO.next()
                        mm(po[:, 0:256], A[:], Vt[:, n, :], True, False, [RA, R_Vt], [Rpo])
                        mm(po[:, 0:256], qf[:], SFs[:, n, :], False, False, [Rqf, R_SFs], [Rpo])
                        mm(po[:, 0:256], qb[:], SBs[:, n, :], False, True, [Rqb, R_SBs], [Rpo])
                        s6, Rs6 = st6.next()
                        DVE(lambda e, s6=s6, po=po: e.bn_stats(out=s6[:], in_=po[:, 0:256]), [Rpo], [Rs6])
                        DVE(lambda e, s6=s6, mv=mv, jj=jj: e.bn_aggr(out=mv[:, jj, 0:2], in_=s6[:]), [Rs6], [Rmv])
                        pos.append((po, Rpo))
                    ACT(lambda e, mv=mv: e.activation(out=mv[:, :, 2], in_=mv[:, :, 1], func=AF.Sqrt, bias=epsc[:, 0:1]), [Rmv, R_epsc], [Rmv])
                    DVE(lambda e, mv=mv: e.reciprocal(out=mv[:, :, 2], in_=mv[:, :, 2]), [Rmv], [Rmv])
                    DVE(lambda e, mv=mv: e.scalar_tensor_tensor(out=mv[:, :, 3], in0=mv[:, :, 0], scalar=-1.0, in1=mv[:, :, 2], op0=ALU.mult, op1=ALU.mult), [Rmv], [Rmv])
                    for jj in range(4):
                        po, Rpo = pos[jj]
                        ACT(lambda e, ret=ret, jj=jj, po=po, mv=mv: e.activation(out=ret[:, jj, :], in_=po[:, 0:256], func=AF.Identity, scale=mv[:, jj, 2:3], bias=mv[:, jj, 3:4]), [Rpo, Rmv], [Rret])
                    og, Rog = RGo.next()
                    DVE(lambda e, og=og, ret=ret, sg=sg: e.tensor_tensor(out=og[:], in0=ret[:], in1=sg[:], op=ALU.mult), [Rret, Rsg], [Rog])
                    DMA(retg_d[n0 * 128:(n0 + 4) * 128, h * 256:(h + 1) * 256].rearrange("(c i) f -> i c f", i=128), og[:], reads=[Rog])
            S.flush()
        if stop_after == "D":
            return nc

        with contextlib.ExitStack() as st:
            T = lambda name, shape, dt=F32: st.enter_context(nc.sbuf_tensor("s_" + name, shape, dt))
            wrnn, R_wrnn = T("wrnn", [128, 8, 1024], BF16), Res()
            wret, R_wret = T("wret", [128, 16, 1024], BF16), Res()
            wout, R_wout = T("wout", [128, 8, 1024], BF16), Res()
            DMA(wrnn[:], w_rnn_d.rearrange("(k p) f -> p k f", p=128), writes=[R_wrnn], q="pool")
            DMA(wret[:, 0:8, :], w_ret_d.rearrange("(k p) f -> p k f", p=128)[:, 0:8, :], writes=[R_wret], q="pool")
            DMA(wret[:, 8:16, :], w_ret_d.rearrange("(k p) f -> p k f", p=128)[:, 8:16, :], writes=[R_wret], q="pool")
            DMA(wout[:], w_out_d.rearrange("(k p) f -> p k f", p=128), writes=[R_wout], q="pool")
            g1bc, R_g1 = T("g1bc", [128, D]), Res()
            A2, R_A2 = T("A2bc", [128, D]), Res()
            B2, R_B2 = T("B2bc", [128, D]), Res()
            n2bc, R_n2 = T("n2bc", [128, D]), Res()
            DMA(g1bc[:], modbc_d[:, 2 * D:3 * D], writes=[R_g1])
            DMA(B2[:], modbc_d[:, 3 * D:4 * D], writes=[R_B2])
            DMA(A2[:], modbc_d[:, 4 * D:5 * D], writes=[R_A2])
            DMA(n2bc[:], n2g_row_d.broadcast_to([128, D]), writes=[R_n2])
            DVE(lambda e: e.scalar_tensor_tensor(out=A2[:], in0=A2[:], scalar=1.0, in1=n2bc[:], op0=ALU.add, op1=ALU.mult), [R_A2, R_n2], [R_A2])
            rgr = Ring(st, nc, "rgin", 2, [128, 8, 512], BF16)
            rtr = Ring(st, nc, "rtin", 2, [128, 4, 2048], BF16)
            rtT, R_rtT = T("rtT", [128, 16, 512], BF16), Res()
            sgr = Ring(st, nc, "sgin", 2, [128, 2, 512], F32)
            mrg, R_mrg = T("mrg", [128, 8, 512], BF16), Res()
            t1r = Ring(st, nc, "t1r", 2, [128, 512], F32)
            t2r = Ring(st, nc, "t2r", 2, [128, 512], F32)
            xr_ = Ring(st, nc, "xtile", 3, [128, D], F32)
            x1r = Ring(st, nc, "x1t", 3, [128, D], F32)
            h2r = Ring(st, nc, "h2t", 2, [128, D], F32)
            sqr = Ring(st, nc, "sqj", 1, [128, D], BF16)
            ssr = Ring(st, nc, "ss2", 4, [128, 2], F32)
            psT = Ring(st, nc, "psT", 2, [128, 512], BF16, psum=True)
            psY = Ring(st, nc, "psY", 4, [128, 512], F32, psum=True)
            rg_v = rgT_d.rearrange("(k p) t -> p k t", p=128)
            def issue_E(tt):
                t0 = tt * 512
                rg, Rrg = rgr.next()
                DMA(rg[:], rg_v[:, :, t0:t0 + 512], writes=[Rrg])
                rt, Rrt = rtr.next()
                DMA(rt[:], retg_d[t0:t0 + 512, :].rearrange("(c i) f -> i c f", i=128), writes=[Rrt])
                return rg, Rrg, rt, Rrt

            def issue_xE(i):
                xt, Rxt = xr_.next()
                DMA(xt[:], x_d[i * 128:(i + 1) * 128, :], writes=[Rxt])
                return xt, Rxt

            nxt_E = issue_E(0)
            nxt_xE = issue_xE(0)
            for tt in range(16):
                t0 = tt * 512
                rg, Rrg, rt, Rrt = nxt_E
                if tt + 1 < 16:
                    nxt_E = issue_E(tt + 1)
                for c in range(16):
                    pt, Rpt = psT.next()
                    for ts in range(4):
                        PE(lambda e, pt=pt, rt=rt, ts=ts, c=c: e.transpose(out=pt[:, ts * 128:(ts + 1) * 128], in_=rt[:, ts, c * 128:(c + 1) * 128], identity=identb[:]), [Rrt, R_identb], [Rpt])
                    if c % 2 == 0:
                        ACT(lambda e, pt=pt, c=c: e.activation(out=rtT[:, c, :], in_=pt[:], func=AF.Copy), [Rpt], [R_rtT])
                    else:
                        DVE(lambda e, pt=pt, c=c: e.tensor_copy(out=rtT[:, c, :], in_=pt[:]), [Rpt], [R_rtT])
                for m in range(8):
                    sg, Rsg = sgr.next()
                    DMA(sg[:], sgT_d[:, m * 128:(m + 1) * 128, t0:t0 + 512].rearrange("c p t -> p c t"), writes=[Rsg])
                    p1, Rp1 = psY.next()
                    for k in range(8):
                        mm(p1[:], wrnn[:, k, m * 128:(m + 1) * 128], rg[:, k, :], k == 0, k == 7, [R_wrnn, Rrg], [Rp1])
                    p2, Rp2 = psY.next()
                    for k in range(16):
                        mm(p2[:], wret[:, k, m * 128:(m + 1) * 128], rtT[:, k, :], k == 0, k == 15, [R_wret, R_rtT], [Rp2])
                    t1, Rt1 = t1r.next()
                    t2, Rt2 = t2r.next()
                    DVE(lambda e, t1=t1, p1=p1, sg=sg: e.tensor_tensor(out=t1[:], in0=p1[:], in1=sg[:, 0, :], op=ALU.mult), [Rp1, Rsg], [Rt1])
                    DVE(lambda e, t2=t2, p2=p2, sg=sg: e.tensor_tensor(out=t2[:], in0=p2[:], in1=sg[:, 1, :], op=ALU.mult), [Rp2, Rsg], [Rt2])
                    POOL(lambda e, t1=t1, t2=t2, m=m: e.tensor_tensor(out=mrg[:, m, :], in0=t1[:], in1=t2[:], op=ALU.add), [Rt1, Rt2], [R_mrg])
                def stE1(ts, tt=tt, t0=t0):
                    nonlocal_x = nxt_xE_box
                    r0 = t0 + ts * 128
                    xt, Rxt = nonlocal_x[0]
                    if tt * 4 + ts + 1 < 64:
                        nonlocal_x[0] = issue_xE(tt * 4 + ts + 1)
                    x1, Rx1 = x1r.next()
                    for mh in range(2):
                        py, Rpy = psY.next()
                        for k in range(8):
                            mm(py[:], mrg[:, k, ts * 128:(ts + 1) * 128], wout[:, k, mh * 512:(mh + 1) * 512], k == 0, k == 7, [R_mrg, R_wout], [Rpy])
                        DVE(lambda e, x1=x1, py=py, mh=mh: e.tensor_tensor(out=x1[:, mh * 512:(mh + 1) * 512], in0=py[:], in1=g1bc[:, mh * 512:(mh + 1) * 512], op=ALU.mult), [Rpy, R_g1], [Rx1])
                    POOL(lambda e, x1=x1, xt=xt: e.tensor_tensor(out=x1[:], in0=x1[:], in1=xt[:], op=ALU.add), [Rx1, Rxt], [Rx1])
                    DMA(x1_d[r0:r0 + 128, :], x1[:], reads=[Rx1])
                    sqt, Rsq = sqr.next()
                    ss, Rss = ssr.next()
                    ACT(lambda e, sqt=sqt, x1=x1, ss=ss: e.activation(out=sqt[:], in_=x1[:], func=AF.Square, accum_out=ss[:, 0:1]), [Rx1], [Rsq, Rss])
                    rstd_from_ss(ss[:, 0:1], ss[:, 1:2], Rss, Rss, D)
                    return x1, Rx1, ss, Rss, r0

                def stE2(st1):
                    x1, Rx1, ss, Rss, r0 = st1
                    h2, Rh2 = h2r.next()
                    DVE(lambda e, h2=h2, x1=x1, ss=ss: e.scalar_tensor_tensor(out=h2[:], in0=x1[:], scalar=ss[:, 1:2], in1=A2[:], op0=ALU.mult, op1=ALU.mult), [Rx1, Rss, R_A2], [Rh2])
                    POOL(lambda e, h2=h2: e.tensor_tensor(out=h2[:], in0=h2[:], in1=B2[:], op=ALU.add), [Rh2, R_B2], [Rh2])
                    DMA(h2_d[r0:r0 + 128, :], h2[:], reads=[Rh2])

                nxt_xE_box = [nxt_xE]
                prev1 = stE1(0)
                for ts in range(4):
                    cur1 = prev1
                    if ts + 1 < 4:
                        prev1 = stE1(ts + 1)
                    stE2(cur1)
                nxt_xE = nxt_xE_box[0]
            S.flush()
        if stop_after == "E":
            return nc

        BIG = float(1 << 22)
        WIDX = top.enter_context(nc.sbuf_tensor("s_WIDX", [128, NBLK, 8], I32)); R_WIDX = Res()
        EBI = top.enter_context(nc.sbuf_tensor("s_EBI", [2, NBLK], I32)); R_EBI = Res()
        WIDX2 = top.enter_context(nc.sbuf_tensor("s_WIDX2", [128, NBLK, 4], I32)); R_WIDX2 = Res()
        with contextlib.ExitStack() as st:
            T = lambda name, shape, dt=F32: st.enter_context(nc.sbuf_tensor("s_" + name, shape, dt))
            rw, R_rw = T("rw", [128, 8, NE]), Res()
            rbbc, R_rb = T("rbbc", [128, NE]), Res()
            tri, R_tri = T("tri", [128, 2, 128]), Res()
            cum, R_cum = T("cum", [128, NE]), Res()
            Lall, R_Lall = T("Lall", [128, 64, NE]), Res()
            T8all, R_T8 = T("T8all", [128, 64, 8]), Res()
            Call, R_Call = T("Call", [128, 64, NE]), Res()
            DMA(rw[:], rw_d.rearrange("(k p) e -> p k e", p=128), writes=[R_rw])
            DMA(rbbc[:], rb_row_d.broadcast_to([128, NE]), writes=[R_rb])
            DMA(tri[:], tri_d.rearrange("c p t -> p c t"), writes=[R_tri])
            POOL(lambda e: e.memset(cum[:], 0.0), [], [R_cum])
            h2r = Ring(st, nc, "h2in", 3, [128, D], F32)
            h2br = Ring(st, nc, "h2b", 3, [128, D], BF16)
            h2Tr = Ring(st, nc, "h2T", 2, [128, 8, 128], F32)
            mkr = Ring(st, nc, "mask", 2, [128, NE], F32)
            Er = Ring(st, nc, "Eexp", 2, [128, NE], F32)
            Gr_ = Ring(st, nc, "Gate", 2, [128, NE], F32)
            dfr = Ring(st, nc, "destf", 2, [128, NE], F32)
            eqr = Ring(st, nc, "eqm", 2, [128, NE], F32)
            jkr = Ring(st, nc, "junk", 2, [128, NE], F32)
            smr = Ring(st, nc, "small", 2, [128, 8], F32)
            psT = Ring(st, nc, "psTf", 2, [128, 4, 128], F32, psum=True)
            psL = Ring(st, nc, "psLg", 2, [128, 512], F32, psum=True)
            psC = Ring(st, nc, "psCn", 2, [128, 512], F32, psum=True)
            for tt in range(64):
                r0 = tt * 128
                h2, Rh2 = h2r.next()
                DMA(h2[:], h2_d[r0:r0 + 128, :], writes=[Rh2])
                hT, RhT = h2Tr.next()
                for half in range(2):
                    pt, Rpt = psT.next()
                    for k4 in range(4):
                        k = half * 4 + k4
                        PE(lambda e, pt=pt, h2=h2, k=k, k4=k4: e.transpose(out=pt[:, k4, :], in_=h2[:, k * 128:(k + 1) * 128], identity=ident[:]), [Rh2, R_ident], [Rpt])
                    DVE(lambda e, hT=hT, pt=pt, half=half: e.tensor_copy(out=hT[:, half * 4:(half + 1) * 4, :], in_=pt[:]), [Rpt], [RhT])
                pl, Rpl = psL.next()
                for k in range(8):
                    mm(pl[:, 0:NE], hT[:, k, :], rw[:, k, :], k == 0, k == 7, [RhT, R_rw], [Rpl])
                Lg = Lall[:, tt, :]
                t8 = T8all[:, tt, :]
                DVE(lambda e, Lg=Lg, pl=pl: e.tensor_tensor(out=Lg, in0=pl[:, 0:NE], in1=rbbc[:], op=ALU.add), [Rpl, R_rb], [R_Lall])
                DVE(lambda e, t8=t8, Lg=Lg: e.max(out=t8, in_=Lg), [R_Lall], [R_T8])
                mk, Rmk = mkr.next()
                DVE(lambda e, mk=mk, Lg=Lg, t8=t8: e.tensor_scalar(out=mk[:], in0=Lg, scalar1=t8[:, 3:4], scalar2=None, op0=ALU.is_ge), [R_Lall, R_T8], [Rmk])
                sm, Rsm = smr.next()
                DVE(lambda e, sm=sm, t8=t8: e.tensor_scalar(out=sm[:, 0:1], in0=t8[:, 0:1], scalar1=-1.0, scalar2=None, op0=ALU.mult), [R_T8], [Rsm])
                Ee, REe = Er.next()
                ACT(lambda e, Ee=Ee, Lg=Lg, sm=sm: e.activation(out=Ee[:], in_=Lg, func=AF.Exp, bias=sm[:, 0:1]), [R_Lall, Rsm], [REe])
                DVE(lambda e, Ee=Ee, mk=mk, sm=sm: e.scalar_tensor_tensor(out=Ee[:], in0=Ee[:], scalar=1.0, in1=mk[:], op0=ALU.mult, op1=ALU.mult, accum_out=sm[:, 1:2]), [REe, Rmk], [REe, Rsm])
                DVE(lambda e, sm=sm: e.reciprocal(out=sm[:, 2:3], in_=sm[:, 1:2]), [Rsm], [Rsm])
                Gt, RGt = Gr_.next()
                DVE(lambda e, Gt=Gt, Ee=Ee, sm=sm: e.tensor_scalar(out=Gt[:], in0=Ee[:], scalar1=sm[:, 2:3], scalar2=None, op0=ALU.mult), [REe, Rsm], [RGt])
                pc, Rpc = psC.next()
                mm(pc[:, 0:NE], tri[:, 0, :], mk[:], True, False, [R_tri, Rmk], [Rpc])
                mm(pc[:, 0:NE], tri[:, 1, :], cum[:], False, True, [R_tri, R_cum], [Rpc])
                ACT(lambda e, pc=pc, tt=tt: e.activation(out=Call[:, tt, :], in_=pc[:, 0:NE], func=AF.Copy), [Rpc], [R_Call])
                POOL(lambda e, mk=mk: e.tensor_tensor(out=cum[:], in0=cum[:], in1=mk[:], op=ALU.add), [R_cum, Rmk], [R_cum])
                for k in range(4):
                    eq, Req = eqr.next()
                    jk, Rjk = jkr.next()
                    DVE(lambda e, eq=eq, Lg=Lg, t8=t8, k=k: e.tensor_scalar(out=eq[:], in0=Lg, scalar1=t8[:, k:k + 1], scalar2=None, op0=ALU.is_equal), [R_Lall, R_T8], [Req])
                    DVE(lambda e, jk=jk, eq=eq, Gt=Gt, tt=tt, k=k: e.scalar_tensor_tensor(out=jk[:], in0=eq[:], scalar=1.0, in1=Gt[:], op0=ALU.mult, op1=ALU.mult, accum_out=GK[:, tt, k:k + 1]), [Req, RGt], [Rjk, R_GK])
            tot, R_tot = T("tot", [128, NE]), Res()
            toti, R_toti = T("toti", [128, NE], I32), Res()
            pend, R_pend = T("pend", [128, NE]), Res()
            pst, R_pst = T("pst", [128, NE]), Res()
            onesE, R_onesE = T("onesE", [128, NE]), Res()
            bvals, R_bv = T("bvals", [128, NBLK]), Res()
            pk, R_pk = T("pk", [128, 8]), Res()
            cmp_, R_cmp = T("cmp", [128, NBLK, NE]), Res()
            eb, R_eb = T("eb", [128, NBLK]), Res()
            ld, R_ld = T("ld", [128, NBLK]), Res()
            vl, R_vl = T("vl", [128, NBLK]), Res()
            wf, R_wf = T("wf", [128, NBLK, 8]), Res()
            DMA(bvals[:], bvals_d.broadcast_to([128, NBLK]), writes=[R_bv])
            DMA(pk[:], pk_d, writes=[R_pk])
            POOL(lambda e: e.memset(onesE[:], 1.0), [], [R_onesE])
            pc, Rpc = psC.next()
            mm(pc[:, 0:NE], tri[:, 1, :], cum[:], True, True, [R_tri, R_cum], [Rpc])
            DVE(lambda e, pc=pc: e.tensor_scalar(out=tot[:], in0=pc[:, 0:NE], scalar1=float(BLK - 1), scalar2=None, op0=ALU.add), [Rpc], [R_tot])
            DVE(lambda e: e.tensor_copy(out=toti[:], in_=tot[:]), [R_tot], [R_toti])
            DVE(lambda e: e.tensor_scalar(out=toti[:], in0=toti[:], scalar1=BLK_SHIFT, scalar2=None, op0=ALU.arith_shift_right), [R_toti], [R_toti])
            DVE(lambda e: e.tensor_scalar(out=toti[:], in0=toti[:], scalar1=BLK_SHIFT, scalar2=None, op0=ALU.logical_shift_left), [R_toti], [R_toti])
            DVE(lambda e: e.tensor_copy(out=tot[:], in_=toti[:]), [R_toti], [R_tot])
            DVE(lambda e: e.tensor_tensor_scan(out=pend[:], data0=onesE[:], data1=tot[:], initial=0.0, op0=ALU.mult, op1=ALU.add), [R_tot, R_onesE], [R_pend])
            DVE(lambda e: e.tensor_tensor(out=pst[:], in0=pend[:], in1=tot[:], op=ALU.subtract), [R_pend, R_tot], [R_pst])
            DVE(lambda e: e.tensor_tensor(out=cmp_[:], in0=pend[:].unsqueeze(1).broadcast_to([128, NBLK, NE]), in1=bvals[:].unsqueeze(2).broadcast_to([128, NBLK, NE]), op=ALU.is_le), [R_pend, R_bv], [R_cmp])
            DVE(lambda e: e.tensor_reduce(out=eb[:], in_=cmp_[:], axis=AX.X, op=ALU.add), [R_cmp], [R_eb])
            POOL(lambda e: e.memset(ld[:, 0:2], 1.0), [], [R_ld])
            DVE(lambda e: e.tensor_tensor(out=ld[:, 2:NBLK], in0=eb[:, 2:NBLK], in1=eb[:, 0:NBLK - 2], op=ALU.not_equal), [R_eb], [R_ld])
            DVE(lambda e: e.tensor_scalar(out=vl[:], in0=eb[:], scalar1=float(NE), scalar2=None, op0=ALU.is_lt), [R_eb], [R_vl])
            DVE(lambda e: e.tensor_tensor(out=ld[:], in0=ld[:], in1=vl[:], op=ALU.mult), [R_ld, R_vl], [R_ld])
            DVE(lambda e: e.tensor_scalar(out=ld[:], in0=ld[:], scalar1=-BIG, scalar2=BIG, op0=ALU.mult, op1=ALU.add), [R_ld], [R_ld])
            DVE(lambda e: e.tensor_tensor(out=vl[:], in0=eb[:], in1=ld[:], op=ALU.add), [R_eb, R_ld], [R_vl])
            DVE(lambda e: e.tensor_copy(out=EBI[0:2, :], in_=vl[0:2, :]), [R_vl], [R_EBI])
            DVE(lambda e: e.scalar_tensor_tensor(out=vl[:], in0=eb[:], scalar=512.0, in1=ld[:], op0=ALU.mult, op1=ALU.add), [R_eb, R_ld], [R_vl])
            DVE(lambda e: e.tensor_tensor(out=wf[:, :, 0:4], in0=vl[:].unsqueeze(2).broadcast_to([128, NBLK, 4]), in1=pk[:, 0:4].unsqueeze(1).broadcast_to([128, NBLK, 4]), op=ALU.add), [R_vl, R_pk], [R_wf])
            DVE(lambda e: e.tensor_copy(out=WIDX2[:], in_=wf[:, :, 0:4]), [R_wf], [R_WIDX2])
            DVE(lambda e: e.scalar_tensor_tensor(out=eb[:], in0=eb[:], scalar=float(D), in1=ld[:], op0=ALU.mult, op1=ALU.add), [R_eb, R_ld], [R_eb])
            DVE(lambda e: e.tensor_tensor(out=wf[:], in0=eb[:].unsqueeze(2).broadcast_to([128, NBLK, 8]), in1=pk[:].unsqueeze(1).broadcast_to([128, NBLK, 8]), op=ALU.add), [R_eb, R_pk], [R_wf])
            DVE(lambda e: e.tensor_copy(out=WIDX[:], in_=wf[:]), [R_wf], [R_WIDX])
            for tt in range(64):
                r0 = tt * 128
                h2, Rh2 = h2r.next()
                DMA(h2[:], h2_d[r0:r0 + 128, :], writes=[Rh2])
                h2b, Rh2b = h2br.next()
                ACT(lambda e, h2b=h2b, h2=h2: e.activation(out=h2b[:], in_=h2[:], func=AF.Copy), [Rh2], [Rh2b])
                df, Rdf = dfr.next()
                DVE(lambda e, df=df, tt=tt: e.tensor_tensor(out=df[:], in0=Call[:, tt, :], in1=pst[:], op=ALU.add), [R_Call, R_pst], [Rdf])
                sm, Rsm = smr.next()
                for k in range(4):
                    eq, Req = eqr.next()
                    jk, Rjk = jkr.next()
                    DVE(lambda e, eq=eq, tt=tt, k=k: e.tensor_scalar(out=eq[:], in0=Lall[:, tt, :], scalar1=T8all[:, tt, k:k + 1], scalar2=None, op0=ALU.is_equal), [R_Lall, R_T8], [Req])
                    DVE(lambda e, jk=jk, eq=eq, df=df, sm=sm, k=k: e.scalar_tensor_tensor(out=jk[:], in0=eq[:], scalar=1.0, in1=df[:], op0=ALU.mult, op1=ALU.mult, accum_out=sm[:, 4 + k:5 + k]), [Req, Rdf], [Rjk, Rsm])
                DVE(lambda e, sm=sm, tt=tt: e.tensor_copy(out=DKi[:, tt, :], in_=sm[:, 4:8]), [Rsm], [R_DKi])
                for k in range(4):
                    S.dma("pool", lambda e, h2b=h2b, tt=tt, k=k: e.indirect_dma_start(out=xs_d, out_offset=bass.IndirectOffsetOnAxis(ap=DKi[:, tt, k:k + 1], axis=0), in_=h2b[:], in_offset=None), reads=[R_DKi, Rh2b])
            if "dkg" in dbg:
                dko, R_dko = T("dko", [128, 64, 8]), Res()
                DVE(lambda e: e.tensor_copy(out=dko[:, :, 0:4], in_=DKi[:]), [R_DKi], [R_dko])
                DVE(lambda e: e.tensor_copy(out=dko[:, :, 4:8], in_=GK[:]), [R_GK], [R_dko])
                DMA(dk_d, dko[:], reads=[R_dko])
            S.flush()
        if stop_after == "F":
            return nc

        with contextlib.ExitStack() as st:
            T = lambda name, shape, dt=F32: st.enter_context(nc.sbuf_tensor("s_" + name, shape, dt))
            w1r = Ring(st, nc, "w1e", 2, [128, 8, 2 * D], BF16)
            w2r = Ring(st, nc, "w2e", 2, [128, 8, D], BF16)
            b12r = Ring(st, nc, "b12e", 2, [2, 3 * D], BF16)
            onesb, R_onesb = T("onesb", [1, 512], BF16), Res()
            POOL(lambda e: e.memset(onesb[:], 1.0), [], [R_onesb])
            onesa, R_onesa = T("onesa", [1, 128], BF16), Res()
            POOL(lambda e: e.memset(onesa[:], 1.702), [], [R_onesa])
            xinr = Ring(st, nc, "xsin", 12, [128, D], BF16)
            xTr = Ring(st, nc, "xTe", 2, [128, 8, BLK], BF16)
            aTr = Ring(st, nc, "aTe", 2, [128, 8, BLK], BF16)
            gr_ = Ring(st, nc, "gte", 2, [128, 512], F32)
            sgr_ = Ring(st, nc, "sge", 2, [128, 512], F32)
            l1r = Ring(st, nc, "l1e", 2, [128, 512], F32)
            ysr = Ring(st, nc, "yse", 3, [128, D], F32)
            psX = Ring(st, nc, "psXe", 2, [128, 8, 128], BF16, psum=True)
            psG = Ring(st, nc, "psGe", 2, [128, 512], F32, psum=True)
            psLn = Ring(st, nc, "psLe", 2, [128, 512], F32, psum=True)
            psY = Ring(st, nc, "psYe", 2, [128, 512], F32, psum=True)
            NSUB = BLK // 128
            breg = nc.gpsimd.alloc_register("bnd_reg")
            S.ops["pool"].append(([], lambda e: e.reg_mov(breg, NE * D - 1), None))
            breg2 = nc.gpsimd.alloc_register("bnd_reg2")
            S.ops["pool"].append(([], lambda e: e.reg_mov(breg2, NE * 512 - 1), None))
            def issue_x(b):
                lst = []
                for s in range(NSUB):
                    xi, Rxi = xinr.next()
                    s0 = b * BLK + s * 128
                    DMA(xi[:], xs_d[s0:s0 + 128, :], writes=[Rxi])
                    lst.append((xi, Rxi))
                return lst

            def xpose(cur_x):
                xT, R_xT = xTr.next()
                for s in range(NSUB):
                    xi, Rxi = cur_x[s]
                    px, Rpx = psX.next()
                    for k in range(8):
                        PE(lambda e, px=px, xi=xi, k=k: e.transpose(out=px[:, k, :], in_=xi[:, k * 128:(k + 1) * 128], identity=identb[:]), [Rxi, R_identb], [Rpx])
                    ACT(lambda e, px=px, xT=xT, s=s: e.activation(out=xT[:, :, s * 128:(s + 1) * 128], in_=px[:], func=AF.Copy), [Rpx], [R_xT])
                return xT, R_xT

            x_q = [issue_x(0), issue_x(1)]
            nxt_xT = None
            for b in range(NBLK):
                w1, Rw1 = w1r.next()
                w2, Rw2 = w2r.next()
                b12, Rb12 = b12r.next()
                b1r, Rb1, b2r, Rb2 = b12, Rb12, b12[:, 2 * D:3 * D], Rb12
                for k in range(8):
                    S.dma("pool", lambda e, b=b, k=k, w1=w1: e.indirect_dma_start(out=w1[:, k, :], out_offset=None, in_=w1_d, in_offset=bass.IndirectOffsetOnAxis(ap=WIDX[:, b, k:k + 1], axis=0), bounds_check=breg, oob_is_err=False), reads=[R_WIDX], writes=[Rw1])
                for k2 in range(4):
                    S.dma("pool", lambda e, b=b, k2=k2, w2=w2: e.indirect_dma_start(out=w2[:].rearrange("p k f -> p (k f)")[:, k2 * 2 * D:(k2 + 1) * 2 * D], out_offset=None, in_=w2_d, in_offset=bass.IndirectOffsetOnAxis(ap=WIDX2[:, b, k2:k2 + 1], axis=0), bounds_check=breg2, oob_is_err=False), reads=[R_WIDX2], writes=[Rw2])
                S.dma("pool", lambda e, b=b, b12=b12: e.indirect_dma_start(out=b12[:], out_offset=None, in_=b12_d, in_offset=bass.IndirectOffsetOnAxis(ap=EBI[0:2, b:b + 1], axis=0), bounds_check=breg, oob_is_err=False), reads=[R_EBI], writes=[Rb12])
                if b == 0:
                    nxt_xT = xpose(x_q.pop(0))
                xT, R_xT = nxt_xT
                if b + 2 < NBLK:
                    x_q.append(issue_x(b + 2))
                aT, R_aT = aTr.next()
                for j in range(8):
                    pg, Rpg = psG.next()
                    pl, Rpl = psLn.next()
                    for k in range(8):
                        mm(pg[:, 0:BLK], w1[:, k, j * 256:(j + 1) * 256:2], xT[:, k, :], k == 0, False, [Rw1, R_xT], [Rpg])
                    mm(pg[:, 0:BLK], b1r[0:1, j * 256:(j + 1) * 256:2], onesb[0:1, 0:BLK], False, True, [Rb1, R_onesb], [Rpg])
                    for k in range(8):
                        mm(pl[:, 0:BLK], w1[:, k, j * 256 + 1:(j + 1) * 256:2], xT[:, k, :], k == 0, False, [Rw1, R_xT], [Rpl])
                    mm(pl[:, 0:BLK], b1r[0:1, j * 256 + 1:(j + 1) * 256:2], onesb[0:1, 0:BLK], False, True, [Rb1, R_onesb], [Rpl])
                    gt, Rgt = gr_.next()
                    sg, Rsg = sgr_.next()
                    l1, Rl1 = l1r.next()
                    m = BLK
                    DVE(lambda e, gt=gt, pg=pg, m=m: e.tensor_scalar(out=gt[:, 0:m], in0=pg[:, 0:m], scalar1=7.0, scalar2=None, op0=ALU.min), [Rpg], [Rgt])
                    ACT(lambda e, sg=sg, gt=gt, m=m: e.activation(out=sg[:, 0:m], in_=gt[:, 0:m], func=AF.Silu, scale=1.702), [Rgt], [Rsg])
                    DVE(lambda e, l1=l1, pl=pl, m=m: e.tensor_scalar(out=l1[:, 0:m], in0=pl[:, 0:m], scalar1=7.0, scalar2=-7.0, op0=ALU.min, op1=ALU.max), [Rpl], [Rl1])
                    DVE(lambda e, aT=aT, sg=sg, l1=l1, j=j, m=m: e.scalar_tensor_tensor(out=aT[:, j, :], in0=l1[:, 0:m], scalar=1.0, in1=sg[:, 0:m], op0=ALU.add, op1=ALU.mult), [Rsg, Rl1], [R_aT])
                if b + 1 < NBLK:
                    nxt_xT = xpose(x_q.pop(0))
                for s in range(NSUB):
                    ys, Rys = ysr.next()
                    for mh in range(2):
                        py, Rpy = psY.next()
                        for j in range(8):
                            mm(py[:], aT[:, j, s * 128:(s + 1) * 128], w2[:, j, mh * 512:(mh + 1) * 512], j == 0, False, [R_aT, Rw2], [Rpy])
                        mm(py[:], onesa[0:1, 0:128], b2r[0:1, mh * 512:(mh + 1) * 512], False, True, [R_onesa, Rb2], [Rpy])
                        ACT(lambda e, ys=ys, py=py, mh=mh: e.activation(out=ys[:, mh * 512:(mh + 1) * 512], in_=py[:], func=AF.Copy, scale=1.0 / 1.702), [Rpy], [Rys])
                    s0 = b * BLK + s * 128
                    DMA(ys_d[s0:s0 + 128, :], ys[:], reads=[Rys])
            S.flush()
        if stop_after == "G":
            return nc

        with contextlib.ExitStack() as st:
            T = lambda name, shape, dt=F32: st.enter_context(nc.sbuf_tensor("s_" + name, shape, dt))
            g2bc, R_g2 = T("g2bc", [128, D]), Res()
            fgbc, R_fg = T("fgbc", [128, D]), Res()
            DMA(g2bc[:], modbc_d[:, 5 * D:6 * D], writes=[R_g2])
            DMA(fgbc[:], fg_row_d.broadcast_to([128, D]), writes=[R_fg])
            ykr = Ring(st, nc, "ykg", 12, [128, D], F32)
            accr = Ring(st, nc, "acc", 3, [128, D], F32)
            x1r = Ring(st, nc, "x1in", 3, [128, D], F32)
            otr = Ring(st, nc, "otile", 2, [128, D], F32)
            sqr = Ring(st, nc, "sqh", 1, [128, D], BF16)
            ssr = Ring(st, nc, "ssh", 4, [128, 2], F32)
            def issue_H(tt):
                yk = []
                for k in range(4):
                    y, Ry = ykr.next()
                    S.dma("pool", lambda e, y=y, tt=tt, k=k: e.indirect_dma_start(out=y[:], out_offset=None, in_=ys_d, in_offset=bass.IndirectOffsetOnAxis(ap=DKi[:, tt, k:k + 1], axis=0)), reads=[R_DKi], writes=[Ry])
                    yk.append((y, Ry))
                x1, Rx1 = x1r.next()
                DMA(x1[:], x1_d[tt * 128:(tt + 1) * 128, :], writes=[Rx1])
                return yk, x1, Rx1

            def stage1_H(tt, item):
                yk, x1, Rx1 = item
                acc, Racc = accr.next()
                DVE(lambda e, acc=acc, y=yk[0][0], tt=tt: e.tensor_scalar(out=acc[:], in0=y[:], scalar1=GK[:, tt, 0:1], scalar2=None, op0=ALU.mult), [yk[0][1], R_GK], [Racc])
                for k in range(1, 4):
                    DVE(lambda e, acc=acc, y=yk[k][0], tt=tt, k=k: e.scalar_tensor_tensor(out=acc[:], in0=y[:], scalar=GK[:, tt, k:k + 1], in1=acc[:], op0=ALU.mult, op1=ALU.add), [yk[k][1], R_GK, Racc], [Racc])
                return acc, Racc, x1, Rx1

            def stage2_H(tt, st1):
                acc, Racc, x1, Rx1 = st1
                r0 = tt * 128
                POOL(lambda e, acc=acc: e.tensor_tensor(out=acc[:], in0=acc[:], in1=g2bc[:], op=ALU.mult), [Racc, R_g2], [Racc])
                POOL(lambda e, acc=acc, x1=x1: e.tensor_tensor(out=acc[:], in0=acc[:], in1=x1[:], op=ALU.add), [Racc, Rx1], [Racc])
                sqt, Rsq = sqr.next()
                ss, Rss = ssr.next()
                ACT(lambda e, sqt=sqt, acc=acc, ss=ss: e.activation(out=sqt[:], in_=acc[:], func=AF.Square, accum_out=ss[:, 0:1]), [Racc], [Rsq, Rss])
                rstd_from_ss(ss[:, 0:1], ss[:, 1:2], Rss, Rss, D)
                ot, Rot = otr.next()
                DVE(lambda e, ot=ot, acc=acc, ss=ss: e.scalar_tensor_tensor(out=ot[:], in0=acc[:], scalar=ss[:, 1:2], in1=fgbc[:], op0=ALU.mult, op1=ALU.mult), [Racc, Rss, R_fg], [Rot])
                DMA(out_d[r0:r0 + 128, :], ot[:], reads=[Rot])

            ld0 = issue_H(0)
            ld1 = issue_H(1)
            s1 = stage1_H(0, ld0)
            for tt in range(64):
                cur = s1
                if tt + 2 < 64:
                    ld2 = issue_H(tt + 2)
                if tt + 1 < 64:
                    s1 = stage1_H(tt + 1, ld1)
                    ld1 = ld2 if tt + 2 < 64 else None
                stage2_H(tt, cur)
            S.flush()
    return nc


_CONST_CACHE = {}


def _const_tables():
    if _CONST_CACHE:
        return _CONST_CACHE
    f8 = np.float64
    t = np.arange(SEQ)
    rows = (t // 64).astype(f8)
    cols = (t % 64).astype(f8)
    inv = 10000.0 ** (-np.arange(32, dtype=f8) / 32.0)
    ang = np.concatenate([rows[:, None] * inv, cols[:, None] * inv], axis=1)
    cos, sin = np.cos(ang), np.sin(ang)
    C = np.concatenate([cos, cos], axis=1)
    Sg = np.concatenate([-sin, sin], axis=1)
    ropeK = np.stack([C, Sg], axis=1).astype(np.float32)
    ropeT = np.stack([C.T, Sg.T], axis=0).astype(np.float32)
    g = 1.0 - 2.0 ** (-5.0 - np.arange(NH, dtype=f8))
    ks = DK ** -0.5
    i = np.arange(128, dtype=f8)
    dec = np.zeros((NH, 3, 128, 128), f8)
    ksc = np.zeros((128, NH, 6), f8)
    for h in range(NH):
        dec[h, 0] = g[h] ** np.abs(i[:, None] - i[None, :]) * ks
        dec[h, 1] = np.broadcast_to(g[h] ** (i + 1.0), (128, 128))
        dec[h, 2] = np.broadcast_to(g[h] ** (128.0 - i), (128, 128))
        ksc[:, h, 0] = g[h] ** (127.0 - i) * ks
        ksc[:, h, 1] = g[h] ** i * ks
        ksc[:, h, 2] = g[h] ** (255.0 - i) * ks
        ksc[:, h, 3] = g[h] ** (127.0 - i) * ks
        ksc[:, h, 4] = g[h] ** i * ks
        ksc[:, h, 5] = g[h] ** (128.0 + i) * ks
    tri = np.zeros((2, 128, 128), np.float32)
    tri[0] = (i[:, None] < i[None, :]).astype(np.float32)
    tri[1] = 1.0
    _CONST_CACHE.update(
        ident=np.eye(128, dtype=np.float32), ropeT=ropeT, ropeK=ropeK,
        dec=dec.astype(np.float32), ksc=ksc.astype(np.float32), tri=tri,
        bvals=(np.arange(NBLK, dtype=np.float32) * BLK).reshape(1, NBLK),
        pk=(np.arange(128, dtype=np.float32)[:, None] + 128.0 * np.arange(8, dtype=np.float32)[None, :]),
        cdec=[float(g[h] ** 128.0) for h in range(NH)],
    )
    return _CONST_CACHE


def _fm(v, nchunk):
    return np.ascontiguousarray(np.asarray(v).reshape(nchunk, 128).T)


def make_in_maps(inp, cores=range(NCORES)):
    ct = _const_tables()
    f32 = np.float32
    shared = dict(
        ada_w=np.ascontiguousarray(inp["ada_w"][0]),
        ada_b_row=np.ascontiguousarray(inp["ada_b"][0].reshape(1, -1)),
        ada_bT=_fm(inp["ada_b"][0], 48),
        n1gT=_fm(inp["norm1_g"][0], 8),
        n2g_row=np.ascontiguousarray(inp["norm2_g"][0].reshape(1, -1)),
        fg_row=np.ascontiguousarray(inp["final_g"].reshape(1, -1)),
        w_in=np.ascontiguousarray(inp["w_in"][0]),
        convT=np.ascontiguousarray(np.concatenate([inp["conv_w"][0], inp["conv_b"][0][None]], axis=0)
                                   .reshape(5, 8, 128).transpose(2, 1, 0)),
        lru_w=np.ascontiguousarray(np.stack([inp["lru_wa"][0], inp["lru_wx"][0]], axis=1)),
        lru_vT=np.ascontiguousarray(np.stack([inp["lru_ba"][0], inp["lru_bx"][0], inp["lru_lambda"][0]], axis=-1)
                                    .reshape(2, 8, 128, 3).transpose(2, 0, 1, 3)),
        w_rnn=np.ascontiguousarray(inp["w_rnn_proj"][0]),
        w_ret=np.ascontiguousarray(inp["w_ret_proj"][0]),
        w_out=np.ascontiguousarray(inp["w_out"][0]),
        router_w=np.ascontiguousarray(inp["router_w"][0]),
        rb_row=np.ascontiguousarray(inp["router_b"][0].reshape(1, -1)),
        moe_w1=np.ascontiguousarray(inp["moe_w1"][0]).reshape(NE * D, 2 * D),
        moe_w2=np.ascontiguousarray(inp["moe_w2"][0].reshape(NE, 4, 2, 128, D).transpose(0, 1, 3, 2, 4)).reshape(NE * 512, 2 * D),
        moe_b12=np.ascontiguousarray(np.concatenate([inp["moe_b1"][0], inp["moe_b2"][0]], axis=1)),
        ident=ct["ident"], ropeT=ct["ropeT"], ropeK=ct["ropeK"], dec=ct["dec"], ksc=ct["ksc"],
        tri=ct["tri"], bvals=ct["bvals"], pk=ct["pk"],
    )
    shared = {k: np.asarray(v, dtype=f32) for k, v in shared.items()}
    maps = []
    for b in cores:
        m = dict(shared)
        m["x"] = np.ascontiguousarray(inp["x"][b], dtype=f32)
        m["ctx"] = np.ascontiguousarray(inp["ctx"][b], dtype=f32)
        m["cT2"] = np.ascontiguousarray(np.stack([_fm(inp["c"][b], 8), _fm(inp["c_ctx"], 8)], axis=-1), dtype=f32)
        maps.append(m)
    return maps


def kernel(**inputs):
    inp = {k: np.asarray(v) for k, v in inputs.items()}
    nc = build_program()
    in_maps = make_in_maps(inp)
    res = run_bass_kernel_spmd(nc, in_maps, core_ids=list(range(NCORES)))
    return np.stack([np.asarray(r["out"], dtype=np.float32) for r in res.results], axis=0)
```
